# Optimizing a Trainium2 kernel written in Bass

```python
import jax, jax.numpy as jnp
from jax import lax
import numpy as np

D_MODEL = 1024
BATCH = 16
SEQ = 2048
DEPTH = 4

RET_HEADS = 4
RET_QK_DIM = 128
RET_V_DIM = 256
RET_CHUNK = 128
SB_HEADS = 8
SB_HEAD_DIM = 128
SB_BLOCK = 128
D_FF = 4 * D_MODEL

ROPE_BASE = 10000.0
NORM_EPS = 1e-6
GN_EPS = 1e-5

RET_QK = RET_HEADS * RET_QK_DIM
RET_V = RET_HEADS * RET_V_DIM
SB_W = SB_HEADS * SB_HEAD_DIM
IN_SIZES = (RET_QK, RET_QK, RET_V, RET_V, SB_W, SB_W, SB_W, D_MODEL, D_MODEL)
D_IN = RET_QK * 2 + RET_V * 2 + SB_W * 3 + D_MODEL * 2

kernel_name = 'hybrid_retention_stickbreaking_gated'


def rmsnorm(x, gain):
    xf = x.astype(jnp.float32)
    y = xf * lax.rsqrt(jnp.mean(xf * xf, axis=-1, keepdims=True) + NORM_EPS)
    return (y * gain.astype(jnp.float32)).astype(x.dtype)


def rotary(t, pos):
    half = t.shape[-1] // 2
    inv_freq = ROPE_BASE ** (-jnp.arange(half, dtype=jnp.float32) / half)
    ang = pos.astype(jnp.float32)[:, None] * inv_freq[None, :]
    cos = jnp.cos(ang)[None, :, None, :]
    sin = jnp.sin(ang)[None, :, None, :]
    t1, t2 = t[..., :half], t[..., half:]
    return jnp.concatenate([t1 * cos - t2 * sin, t1 * sin + t2 * cos], axis=-1)


def retention(q, k, v, log_gamma):
    B, S, H, _ = q.shape
    C = RET_CHUNK
    N = S // C
    idx = jnp.arange(C, dtype=jnp.float32)
    diff = idx[:, None] - idx[None, :]
    decay = jnp.where(diff[None] >= 0,
                      jnp.exp(log_gamma[:, None, None] * jnp.maximum(diff, 0.0)[None]), 0.0)
    q_decay = jnp.exp(log_gamma[None, :] * (idx[:, None] + 1.0))
    k_decay = jnp.exp(log_gamma[None, :] * (C - 1.0 - idx[:, None]))
    chunk_decay = jnp.exp(log_gamma * C)

    def to_chunks(t):
        return t.reshape(B, N, C, H, t.shape[-1]).transpose(1, 0, 2, 3, 4)

    def step(state, qkv):
        qc, kc, vc = qkv
        scores = jnp.einsum('bchd,bshd->bhcs', qc, kc) * decay[None]
        inner = jnp.einsum('bhcs,bshe->bche', scores, vc)
        cross = jnp.einsum('bchd,bhde->bche', qc, state) * q_decay[None, :, :, None]
        state = state * chunk_decay[None, :, None, None] + jnp.einsum(
            'bshd,bshe->bhde', kc * k_decay[None, :, :, None], vc)
        return state, inner + cross

    init = jnp.zeros((B, H, q.shape[-1], v.shape[-1]), jnp.float32)
    _, out = lax.scan(step, init, (to_chunks(q), to_chunks(k), to_chunks(v)))
    return out.transpose(1, 0, 2, 3, 4).reshape(B, S, H, v.shape[-1])


def head_group_norm(y):
    mu = jnp.mean(y, axis=-1, keepdims=True)
    var = jnp.mean(jnp.square(y - mu), axis=-1, keepdims=True)
    return (y - mu) * lax.rsqrt(var + GN_EPS)


def stick_breaking(q, k, v):
    S = q.shape[1]
    scale = q.shape[-1] ** -0.5
    outs = []
    for start in range(0, S, SB_BLOCK):
        end = start + SB_BLOCK
        qb = q[:, start:end]
        kb = k[:, :end]
        vb = v[:, :end]
        z = jnp.einsum('bthd,bshd->bhts', qb, kb) * scale
        t_idx = start + jnp.arange(SB_BLOCK)
        s_idx = jnp.arange(end)
        causal = (s_idx[None, :] < t_idx[:, None])[None, None]
        log_beta = jax.nn.log_sigmoid(z)
        log_1m_beta = jnp.where(causal, log_beta - z, 0.0)
        after = lax.cumsum(log_1m_beta, axis=3, reverse=True) - log_1m_beta
        weights = jnp.where(causal, jnp.exp(log_beta + after), 0.0)
        outs.append(jnp.einsum('bhts,bshe->bthe', weights, vb))
    return jnp.concatenate(outs, axis=1)


def setup_inputs(seed: int = 0) -> dict:
    key = jax.random.key(seed)
    ks = jax.random.split(key, 10)
    f32 = jnp.float32
    x = jax.random.normal(ks[0], (BATCH, SEQ, D_MODEL), f32)
    w_in = jax.random.normal(ks[1], (DEPTH, D_MODEL, D_IN), f32) * D_MODEL ** -0.5
    p_ret = jax.random.normal(ks[2], (DEPTH, RET_V, D_MODEL), f32) * RET_V ** -0.5
    p_sb = jax.random.normal(ks[3], (DEPTH, SB_W, D_MODEL), f32) * SB_W ** -0.5
    w_out = jax.random.normal(ks[4], (DEPTH, D_MODEL, D_MODEL), f32) * D_MODEL ** -0.5
    w_up = jax.random.normal(ks[5], (DEPTH, D_MODEL, D_FF), f32) * D_MODEL ** -0.5
    w_down = jax.random.normal(ks[6], (DEPTH, D_FF, D_MODEL), f32) * D_FF ** -0.5
    g_mix = 1.0 + 0.02 * jax.random.normal(ks[7], (DEPTH, D_MODEL), f32)
    g_mlp = 1.0 + 0.02 * jax.random.normal(ks[8], (DEPTH, D_MODEL), f32)
    g_final = 1.0 + 0.02 * jax.random.normal(ks[9], (D_MODEL,), f32)
    return {'x': x, 'w_in': w_in, 'p_ret': p_ret, 'p_sb': p_sb, 'w_out': w_out,
            'w_up': w_up, 'w_down': w_down, 'g_mix': g_mix, 'g_mlp': g_mlp,
            'g_final': g_final}


def reference(x, w_in, p_ret, p_sb, w_out, w_up, w_down, g_mix, g_mlp, g_final):
    B, S, _ = x.shape
    dt = x.dtype
    pos = jnp.arange(S, dtype=jnp.int32)
    log_gamma = jnp.log1p(-(2.0 ** (-5.0 - jnp.arange(RET_HEADS, dtype=jnp.float32))))
    splits = [int(v) for v in np.cumsum(IN_SIZES)[:-1]]
    for l in range(DEPTH):
        h = rmsnorm(x, g_mix[l])
        proj = jnp.matmul(h, w_in[l]).astype(jnp.float32)
        q_r, k_r, v_r, g_r, q_s, k_s, v_s, gate_a, gate_b = jnp.split(proj, splits, axis=-1)
        q_r = rotary(q_r.reshape(B, S, RET_HEADS, RET_QK_DIM), pos)
        k_r = rotary(k_r.reshape(B, S, RET_HEADS, RET_QK_DIM), pos) * (RET_QK_DIM ** -0.5)
        ret = retention(q_r, k_r, v_r.reshape(B, S, RET_HEADS, RET_V_DIM), log_gamma)
        ret = head_group_norm(ret).reshape(B, S, RET_V) * jax.nn.silu(g_r)
        sb = stick_breaking(q_s.reshape(B, S, SB_HEADS, SB_HEAD_DIM),
                            k_s.reshape(B, S, SB_HEADS, SB_HEAD_DIM),
                            v_s.reshape(B, S, SB_HEADS, SB_HEAD_DIM)).reshape(B, S, SB_W)
        branch_a = jnp.matmul(ret.astype(dt), p_ret[l]).astype(jnp.float32)
        branch_b = jnp.matmul(sb.astype(dt), p_sb[l]).astype(jnp.float32)
        merged = jax.nn.sigmoid(gate_a) * branch_a + jax.nn.sigmoid(gate_b) * branch_b
        x = x + jnp.matmul(merged.astype(dt), w_out[l]).astype(dt)
        h = rmsnorm(x, g_mlp[l])
        up = jnp.square(jax.nn.relu(jnp.matmul(h, w_up[l])))
        x = x + jnp.matmul(up, w_down[l]).astype(dt)
    return rmsnorm(x, g_final)
```

```python
import contextlib
import numpy as np
import concourse.bass as bass
import concourse.mybir as mybir
from concourse.bass_utils import run_bass_kernel_spmd

F32 = mybir.dt.float32
BF16 = mybir.dt.bfloat16
ALU = mybir.AluOpType
AF = mybir.ActivationFunctionType
AX = mybir.AxisListType

PE, ACT, DVE, POOL, SP = "pe", "act", "dve", "pool", "sp"
SEM_LIMIT = 30000
DMA_LIMIT = 1800


class V:
    __slots__ = ("ap", "buf", "p0", "p1", "lo", "hi")

    def __init__(self, ap, buf, p0, p1, lo, hi):
        self.ap, self.buf, self.p0, self.p1, self.lo, self.hi = ap, buf, p0, p1, lo, hi

    def re(self, pattern, **kw):
        return V(self.ap.rearrange(pattern, **kw), self.buf, self.p0, self.p1, self.lo, self.hi)

    def with_ap(self, ap):
        return V(ap, self.buf, self.p0, self.p1, self.lo, self.hi)


class Buf:
    def __init__(self, name, handle, P, F, dtype, is_psum=False):
        self.name, self.h, self.P, self.F, self.dtype = name, handle, P, F, dtype
        self.is_psum = is_psum
        self.recs = []

    def v(self, lo=0, hi=None, p0=0, p1=None, f32=False):
        if hi is None:
            hi = self.F
        if p1 is None:
            p1 = self.P
        assert 0 <= lo < hi <= self.F and 0 <= p0 < p1 <= self.P, (self.name, lo, hi, p0, p1)
        ap = self.h[p0:p1, lo:hi]
        if f32:
            assert lo % 2 == 0 and hi % 2 == 0
            ap = ap.bitcast(F32)
        return V(ap, self, p0, p1, lo, hi)


class DmaSem:
    def __init__(self, sched, name):
        self.sched, self.name = sched, name
        self.hw = [sched._new_sem(name + "_0")]
        self.count = 0
        self.finals = []
        self.consumers = []


class Op:
    __slots__ = ("eng", "fn", "deps", "dsem", "dsem_hw", "dsem_val", "signal", "sigval", "is_dma")

    def __init__(self, eng, fn, deps, dsem=None):
        self.eng, self.fn, self.deps, self.dsem = eng, fn, deps, dsem
        self.is_dma = dsem is not None
        self.signal = False
        self.sigval = None
        self.dsem_hw = None
        self.dsem_val = None


class Sched:
    def __init__(self, nc, stack):
        self.nc, self.stack = nc, stack
        self.ops = []
        self.bufs = []
        self.nsem = 0
        self.final_waits = []

    def _new_sem(self, name):
        self.nsem += 1
        return self.stack.enter_context(self.nc.semaphore(name))

    def sbuf(self, name, P, F, dtype):
        h = self.stack.enter_context(self.nc.sbuf_tensor(name, [P, F], dtype))
        b = Buf(name, h, P, F, dtype)
        self.bufs.append(b)
        return b

    def psum(self, name, P, F, dtype):
        h = self.stack.enter_context(self.nc.psum_tensor(name, [P, F], dtype))
        b = Buf(name, h, P, F, dtype, is_psum=True)
        self.bufs.append(b)
        return b

    def dma_sem(self, name):
        return DmaSem(self, name)

    def _access(self, v, n, eng, is_write, is_dma):
        deps = set()
        recs = v.buf.recs
        keep = []
        psum = v.buf.is_psum
        if psum:
            v = V(v.ap, v.buf, 0, v.buf.P, 0, v.buf.F)
        for r in recs:
            ov = not (r[1] <= v.p0 or v.p1 <= r[0] or r[3] <= v.lo or v.hi <= r[2])
            if ov and (r[5] or is_write or (psum and r[6] != eng)) and r[4] != n:
                deps.add(r[4])
            covered = ov and v.p0 <= r[0] and r[1] <= v.p1 and v.lo <= r[2] and r[3] <= v.hi
            if covered and r[4] != n:
                if is_write:
                    continue
                if (not r[5]) and (not is_dma) and r[6] == eng:
                    continue
            keep.append(r)
        keep.append([v.p0, v.p1, v.lo, v.hi, n, is_write, None if is_dma else eng])
        v.buf.recs = keep
        return deps

    def op(self, eng, fn, reads=(), writes=(), dsem=None):
        n = len(self.ops)
        is_dma = dsem is not None
        deps = set()
        for v in reads:
            deps |= self._access(v, n, eng, False, is_dma)
        for v in writes:
            deps |= self._access(v, n, eng, True, is_dma)
        deps.discard(n)
        o = Op(eng, fn, None, dsem)
        if is_dma:
            if dsem.consumers:
                deps |= set(dsem.consumers)
                dsem.consumers = []
                if dsem.count > DMA_LIMIT:
                    dsem.finals.append(dsem.count * 16)
                    dsem.hw.append(self._new_sem(dsem.name + "_%d" % len(dsem.hw)))
                    dsem.count = 0
            dsem.count += 1
            o.dsem_hw = len(dsem.hw) - 1
            o.dsem_val = dsem.count * 16
        final = []
        for d in deps:
            p = self.ops[d]
            if p.is_dma:
                if p.dsem_hw != len(p.dsem.hw) - 1:
                    final.append(("d", p.dsem, p.dsem_hw, p.dsem.finals[p.dsem_hw]))
                    continue
                final.append(("d", p.dsem, p.dsem_hw, p.dsem.count * 16 if p.dsem is not dsem else p.dsem_val))
                if p.dsem is not dsem:
                    p.dsem.consumers.append(n)
            else:
                if p.eng == PE and eng == PE:
                    continue
                p.signal = True
                final.append(("e", d))
        o.deps = final
        self.ops.append(o)
        return n

    def pe(self, fn, reads=(), writes=()):
        return self.op(PE, fn, reads, writes)

    def act(self, fn, reads=(), writes=()):
        return self.op(ACT, fn, reads, writes)

    def dve(self, fn, reads=(), writes=()):
        return self.op(DVE, fn, reads, writes)

    def pool(self, fn, reads=(), writes=()):
        return self.op(POOL, fn, reads, writes)

    def dma(self, eng, dsem, out, in_, reads=(), writes=()):
        return self.op(eng, lambda e: e.dma_start(out=out, in_=in_), reads, writes, dsem=dsem)

    def emit(self, out_sems=()):
        nc = self.nc
        engs = [PE, ACT, DVE, POOL, SP]
        counts = {e: 0 for e in engs}
        for o in self.ops:
            if o.signal and not o.is_dma:
                counts[o.eng] += 1
                o.sigval = counts[o.eng]
        esems = {}
        for e in engs:
            nchunks = (counts[e] + SEM_LIMIT - 1) // SEM_LIMIT
            esems[e] = [self._new_sem("sig_%s_%d" % (e, i)) for i in range(max(nchunks, 1))]
        per_eng = {e: [] for e in engs}
        for o in self.ops:
            per_eng[o.eng].append(o)
        ops = self.ops
        stats = {e: [len(per_eng[e]), 0] for e in engs}

        def replay(eng_name, eobj):
            waited = {}
            for o in per_eng[eng_name]:
                need = {}
                for d in o.deps:
                    if d[0] == "d":
                        key = ("d", id(d[1]), d[2])
                        sem = d[1].hw[d[2]]
                        val = d[3]
                    else:
                        p = ops[d[1]]
                        ch = (p.sigval - 1) // SEM_LIMIT
                        key = ("e", p.eng, ch)
                        sem = esems[p.eng][ch]
                        val = (p.sigval - 1) % SEM_LIMIT + 1
                    if waited.get(key, 0) >= val:
                        continue
                    if key not in need or need[key][1] < val:
                        need[key] = (sem, val)
                for key, (sem, val) in need.items():
                    eobj.wait_ge(sem, val)
                    waited[key] = val
                    stats[eng_name][1] += 1
                ins = o.fn(eobj)
                if o.is_dma:
                    ins.then_inc(o.dsem.hw[o.dsem_hw], 16)
                elif o.signal:
                    ch = (o.sigval - 1) // SEM_LIMIT
                    ins.then_inc(esems[o.eng][ch], 1)
            if eng_name == SP:
                for ds in out_sems:
                    eobj.wait_ge(ds.hw[-1], ds.count * 16)

        with nc.Block() as block:
            @block.tensor
            def _(e):
                replay(PE, e)

            @block.scalar
            def _(e):
                replay(ACT, e)

            @block.vector
            def _(e):
                replay(DVE, e)

            @block.gpsimd
            def _(e):
                replay(POOL, e)

            @block.sync
            def _(e):
                replay(SP, e)
        return stats


D = 1024
SEQ = 2048
DEPTH = 4
NCORES = 8
NG = 42
GW = 4096
EPS = 1e-6
GN_EPS = 1e-5
T = 512
GAMMAS = [1.0 - 2.0 ** (-5.0 - h) for h in range(4)]

X_OFF = 0
HT_OFF = 32768
R1_OFF = 49152
R2_OFF = 65536
W_OFF = [81920, 86016]
SCR = 90112
CST = 104960
ARENA = 105984
C_GMIX, C_GMLP, C_GFIN, C_KDEC, C_QDEC, NSM = 0, 32, 64, 72, 76, 80
TRI_OFF, ONES_OFF, ID_OFF = CST + 160, CST + 288, CST + 416


def build(NL, NSEQ, stop=None):
    nc = bass.Bass("TRN2", target_bir_lowering=False)
    xin = nc.dram_tensor("xin", [NSEQ, 128, 8 * SEQ], F32, kind="ExternalInput").ap()
    wall = nc.dram_tensor("wall", [NL, NG, 128, GW], F32, kind="ExternalInput").ap()
    ctab = nc.dram_tensor("ctab", [128, 2 * SEQ], F32, kind="ExternalInput").ap()
    cdt = nc.dram_tensor("cdt", [128, 4 * 512], F32, kind="ExternalInput").ap()
    cmask = nc.dram_tensor("cmask", [128, 4 * 512], F32, kind="ExternalInput").ap()
    csm = nc.dram_tensor("csm", [128, NSM], F32, kind="ExternalInput").ap()
    ctri = nc.dram_tensor("ctri", [128, 3 * 128], F32, kind="ExternalInput").ap()
    out = nc.dram_tensor("out", [NSEQ, 128, 8 * SEQ], F32, kind="ExternalOutput").ap()
    dbg = nc.dram_tensor("dbg", [128, ARENA], BF16, kind="ExternalOutput").ap() if stop is not None else None

    with contextlib.ExitStack() as st:
        S = Sched(nc, st)
        A = S.sbuf("arena", 128, ARENA, BF16)
        PB = [S.psum("pb%d" % i, 128, 512, F32) for i in range(7)]
        PT = S.psum("pt", 128, 1024, BF16)
        rot = [0]

        def rb():
            rot[0] = (rot[0] + 1) % 5
            return PB[rot[0]]

        wsem = [S.dma_sem("w0"), S.dma_sem("w1")]
        xsem = S.dma_sem("x")
        csem = S.dma_sem("c")
        tsem = [S.dma_sem("t0"), S.dma_sem("t1")]
        osem = S.dma_sem("o")

        def bv(off, n):
            return A.v(off, off + n)

        def fv(off, n):
            return A.v(off, off + 2 * n, f32=True)

        def Xv(k, c0, n):
            return fv(X_OFF + 2 * (k * SEQ + c0), n)

        def hT(k, c0, n):
            return bv(HT_OFF + k * SEQ + c0, n)

        def cs(col):
            return fv(CST + 2 * col, 1)

        TRI = bv(TRI_OFF, 128)
        ONES = bv(ONES_OFF, 128)
        IDENT = bv(ID_OFF, 128)

        def mm(ps, lhsT, rhs, start, stop):
            S.pe(lambda e: e.matmul(ps.ap, lhsT=lhsT.ap, rhs=rhs.ap, start=start, stop=stop),
                 reads=[lhsT, rhs], writes=[ps])

        def tr(ps, in_):
            S.pe(lambda e: e.transpose(ps.ap, in_.ap, IDENT.ap), reads=[in_, IDENT], writes=[ps])

        def act(o, i, func, scale=1.0, bias=0.0):
            reads = [i]
            sc, bi = scale, bias
            if isinstance(scale, V):
                reads.append(scale)
                sc = scale.ap
            if isinstance(bias, V):
                reads.append(bias)
                bi = bias.ap
            S.act(lambda e: e.activation(out=o.ap, in_=i.ap, func=func, scale=sc, bias=bi), reads=reads, writes=[o])

        def tt(o, a, b, op):
            S.dve(lambda e: e.tensor_tensor(out=o.ap, in0=a.ap, in1=b.ap, op=op), reads=[a, b], writes=[o])

        def ts(o, a, s1, s2, op0, op1=None):
            reads = [a]
            v1, v2 = s1, s2
            if isinstance(s1, V):
                reads.append(s1)
                v1 = s1.ap
            if isinstance(s2, V):
                reads.append(s2)
                v2 = s2.ap
            if op1 is None:
                S.dve(lambda e: e.tensor_scalar(out=o.ap, in0=a.ap, scalar1=v1, scalar2=None, op0=op0),
                      reads=reads, writes=[o])
            else:
                S.dve(lambda e: e.tensor_scalar(out=o.ap, in0=a.ap, scalar1=v1, scalar2=v2, op0=op0, op1=op1),
                      reads=reads, writes=[o])

        def stt(o, a, sc, b, op0, op1):
            reads = [a, b]
            v = sc
            if isinstance(sc, V):
                reads.append(sc)
                v = sc.ap
            S.dve(lambda e: e.scalar_tensor_tensor(out=o.ap, in0=a.ap, scalar=v, in1=b.ap, op0=op0, op1=op1),
                  reads=reads, writes=[o])

        def cpy(o, i):
            S.dve(lambda e: e.tensor_copy(out=o.ap, in_=i.ap), reads=[i], writes=[o])

        def sigmoid_to(o, ps, t1, t2):
            act(t1, ps, AF.Exp, scale=-1.0)
            act(t2, t1, AF.Ln, bias=1.0)
            act(o, t2, AF.Exp, scale=-1.0)

        wstate = {"next": 0, "list": []}
        for l in range(NL):
            for g in range(NG):
                wstate["list"].append((l, g))
        wstate["list"] = wstate["list"] * NSEQ
        nW = len(wstate["list"])
        wcnt = [0]

        def issue_w(i):
            l, g = wstate["list"][i]
            sl = i % 2
            dst = bv(W_OFF[sl], GW)
            S.dma(POOL, wsem[sl], dst.ap, wall[l, g], writes=[dst])

        def next_w():
            i = wcnt[0]
            if i == 0:
                issue_w(0)
            if i + 1 < nW:
                issue_w(i + 1)
            wcnt[0] += 1
            return W_OFF[i % 2]

        d0 = fv(CST, NSM)
        S.dma(SP, csem, d0.ap, csm, writes=[d0])
        d1 = bv(TRI_OFF, 384)
        S.dma(POOL, csem, d1.ap, ctri, writes=[d1])

        def rmsnorm(gbase, dst_fn, after=None):
            sqb = lambda k: bv(SCR + k * 512, 512)
            lnt = fv(SCR + 4096, 512)
            rstd = fv(SCR + 5120, 512)
            for t in range(4):
                c0 = t * 512
                for k in range(8):
                    act(sqb(k), Xv(k, c0, 512), AF.Square)
                ps = rb().v()
                for k in range(8):
                    mm(ps, ONES, sqb(k), k == 0, k == 7)
                act(lnt, ps, AF.Ln, scale=1.0 / D, bias=EPS)
                act(rstd, lnt, AF.Exp, scale=-0.5)
                for k in range(8):
                    stt(dst_fn(k, t), Xv(k, c0, 512), cs(gbase + k), rstd, ALU.mult, ALU.mult)
                if after is not None:
                    after(t)

        def retention_head(hh):
            qT = lambda c0, n: bv(R2_OFF + c0, n)
            kT = lambda c0, n: bv(R2_OFF + 2048 + c0, n)
            ktok = lambda n0, nn: bv(R2_OFF + 4096 + n0 * 128, nn * 128)
            vtok = lambda n0, nn: bv(R2_OFF + 6144 + n0 * 256, nn * 256)
            sg = lambda n0, nn: bv(R2_OFF + 10240 + n0 * 256, nn * 256)
            y4 = lambda j0, nj: fv(R2_OFF + 14336 + 2 * j0 * 256, nj * 256)
            cosb = lambda i: fv(SCR + i * 1024, 512)
            sinb = lambda i: fv(SCR + 2048 + i * 1024, 512)
            DT = fv(SCR + 4096, 512)
            tmp1 = fv(SCR + 5120, 512)
            tmp2 = fv(SCR + 6144, 512)
            Sf = lambda i: fv(SCR + 7168 + i * 512, 256)
            Sb = lambda n: bv(SCR + 8192 + n * 256, 256)
            scT = lambda i: bv(SCR + 12288 + i * 512, 512)
            rtok = bv(SCR + 13312, 1024)
            bnst = lambda j: fv(SCR + 14336 + j * 12, 6)
            mv = lambda j: fv(SCR + 14336 + 48 + j * 4, 2)
            mvall = fv(SCR + 14336 + 48, 8)
            lnv = fv(SCR + 14336 + 64, 4)
            rs4 = fv(SCR + 14336 + 72, 4)
            kdec = cs(C_KDEC + hh)
            qdec = cs(C_QDEC + hh)
            gC = GAMMAS[hh] ** 128

            S.dma(SP, csem, DT.ap, cdt[:, hh * 512:(hh + 1) * 512], writes=[DT])
            w = next_w()
            wv = lambda k, c0, n: bv(w + k * 512 + c0, n)
            for t in range(4):
                c0 = t * 512
                cb, sb_ = cosb(t % 2), sinb(t % 2)
                S.dma(SP, tsem[t % 2], cb.ap, ctab[:, c0:c0 + 512], writes=[cb])
                S.dma(SP, tsem[t % 2], sb_.ap, ctab[:, SEQ + c0:SEQ + c0 + 512], writes=[sb_])
                for dst, col in ((qT, 0), (kT, 256)):
                    ps1 = rb().v()
                    for k in range(8):
                        mm(ps1, wv(k, col, 128), hT(k, c0, 512), k == 0, k == 7)
                    ps2 = rb().v()
                    for k in range(8):
                        mm(ps2, wv(k, col + 128, 128), hT(k, c0, 512), k == 0, k == 7)
                    tt(tmp1, ps1, cb, ALU.mult)
                    tt(tmp2, ps2, sb_, ALU.mult)
                    tt(dst(c0, 512), tmp1, tmp2, ALU.add)
            w = next_w()
            for t in range(4):
                for pr in range(2):
                    n0 = t * 4 + pr * 2
                    psv = rb()
                    for j in range(2):
                        o = psv.v(j * 256, (j + 1) * 256)
                        for k in range(8):
                            mm(o, hT(k, (n0 + j) * 128, 128), wv(k, 0, 256), k == 0, k == 7)
                    act(vtok(n0, 2), psv.v(), AF.Copy)
                    psg = rb()
                    for j in range(2):
                        o = psg.v(j * 256, (j + 1) * 256)
                        for k in range(8):
                            mm(o, hT(k, (n0 + j) * 128, 128), wv(k, 256, 256), k == 0, k == 7)
                    sigmoid_to(tmp1, psg.v(), tmp1, tmp2)
                    tt(sg(n0, 2), psg.v(), tmp1, ALU.mult)
            for t in range(4):
                for j in range(4):
                    tr(PT.v(j * 128, (j + 1) * 128), kT((t * 4 + j) * 128, 128))
                act(ktok(t * 4, 4), PT.v(0, 512), AF.Copy, scale=kdec)
            S.dve(lambda e: e.memset(Sb(0).ap, 0.0), writes=[Sb(0)])
            for n in range(15):
                ps = rb().v(0, 256)
                mm(ps, ktok(n, 1), vtok(n, 1), True, True)
                if n == 0:
                    cpy(Sf(0), ps)
                else:
                    stt(Sf(n % 2), Sf((n - 1) % 2), gC, ps, ALU.mult, ALU.add)
                act(Sb(n + 1), Sf(n % 2), AF.Copy)
            for t in range(4):
                pss = rb()
                for j in range(4):
                    n = t * 4 + j
                    mm(pss.v(j * 128, (j + 1) * 128), kT(n * 128, 128), qT(n * 128, 128), True, True)
                sc = scT(t % 2)
                tt(sc, pss.v(), DT, ALU.mult)
                for pr in range(2):
                    pso, psx = rb(), rb()
                    for j in range(2):
                        jj = pr * 2 + j
                        n = t * 4 + jj
                        mm(pso.v(j * 256, (j + 1) * 256), bv(SCR + 12288 + (t % 2) * 512 + jj * 128, 128), vtok(n, 1), True, True)
                        mm(psx.v(j * 256, (j + 1) * 256), qT(n * 128, 128), Sb(n), True, True)
                    act(tmp1, psx.v(), AF.Copy, scale=qdec)
                    tt(y4(pr * 2, 2), pso.v(), tmp1, ALU.add)
                for j in range(4):
                    S.dve(lambda e, j=j: e.bn_stats(out=bnst(j).ap, in_=y4(j, 1).ap), reads=[y4(j, 1)], writes=[bnst(j)])
                    S.dve(lambda e, j=j: e.bn_aggr(out=mv(j).ap, in_=bnst(j).ap), reads=[bnst(j)], writes=[mv(j)])
                mvar = mvall.with_ap(mvall.ap.rearrange("p (j c) -> p j c", c=2)[:, :, 1])
                act(lnv, mvar, AF.Ln, bias=GN_EPS)
                act(rs4, lnv, AF.Exp, scale=-0.5)
                for j in range(4):
                    n = t * 4 + j
                    tj = fv(SCR + 5120 + (j % 2) * 1024, 256)
                    ts(tj, y4(j, 1), fv(SCR + 14336 + 48 + j * 4, 1), fv(SCR + 14336 + 72 + j * 2, 1), ALU.subtract, ALU.mult)
                    tt(bv(SCR + 13312 + j * 256, 256), tj, sg(n, 1), ALU.mult)
                for e2 in range(2):
                    for j in range(4):
                        tr(PT.v((e2 * 4 + j) * 128, (e2 * 4 + j + 1) * 128), bv(SCR + 13312 + j * 256 + e2 * 128, 128))
                for e2 in range(2):
                    act(bv(R1_OFF + (2 * hh + e2) * SEQ + t * 512, 512), PT.v(e2 * 512, (e2 + 1) * 512), AF.Copy)

        def gated_proj(src_off, first):
            e1 = fv(SCR, 512)
            e2 = fv(SCR + 1024, 512)
            sgm = lambda i: fv(SCR + 2048 + i * 1024, 512)
            tb = fv(SCR + 4096, 512)
            for g in range(4):
                w = next_w()
                wv = lambda k, c0, n: bv(w + k * 512 + c0, n)
                for cc in range(2):
                    c = g * 2 + cc
                    for t in range(4):
                        psa, psg = rb().v(), rb().v()
                        for k in range(8):
                            mm(psa, wv(k, cc * 128, 128), bv(src_off + k * SEQ + t * 512, 512), k == 0, k == 7)
                        for k in range(8):
                            mm(psg, wv(k, 256 + cc * 128, 128), hT(k, t * 512, 512), k == 0, k == 7)
                        sm = sgm(t % 2)
                        sigmoid_to(sm, psg, e1, e2)
                        ga = bv(R2_OFF + c * SEQ + t * 512, 512)
                        if first:
                            tt(ga, psa, sm, ALU.mult)
                        else:
                            tt(tb, psa, sm, ALU.mult)
                            tt(ga, ga, tb, ALU.add)

        def sb_head(hh):
            qT = lambda c0, n: bv(SCR + c0, n)
            kT = lambda c0, n: bv(SCR + 2048 + c0, n)
            vtok = lambda n0, nn: bv(SCR + 4096 + n0 * 128, nn * 128)
            nq = bv(SCR + 6144, 512)
            E = fv(SCR + 6656, 512)
            Lp = lambda i: bv(SCR + 7680 + i * 512, 512)
            tmp = lambda i: fv(SCR + 8704 + i * 1024, 512)
            Wt = lambda i: bv(SCR + 10752 + i * 512, 512)
            R = fv(SCR + 11776, 512)
            M = lambda j: bv(SCR + 12800 + j * 512, 512)
            scale = 128.0 ** -0.5
            w = next_w()
            wv = lambda k, c0, n: bv(w + k * 384 + c0, n)
            for t in range(4):
                psq = rb().v()
                for k in range(8):
                    mm(psq, wv(k, 0, 128), hT(k, t * 512, 512), k == 0, k == 7)
                act(qT(t * 512, 512), psq, AF.Copy, scale=scale)
                psk = rb().v()
                for k in range(8):
                    mm(psk, wv(k, 128, 128), hT(k, t * 512, 512), k == 0, k == 7)
                act(kT(t * 512, 512), psk, AF.Copy)
                psv = rb()
                for j in range(4):
                    o = psv.v(j * 128, (j + 1) * 128)
                    for k in range(8):
                        mm(o, hT(k, (t * 4 + j) * 128, 128), wv(k, 256, 128), k == 0, k == 7)
                cpy(vtok(t * 4, 4), psv.v())
            it = 0
            for i in range(4):
                pso = PB[5 + (i % 2)].v()
                ts(nq, qT(i * 512, 512), -1.0, None, ALU.mult)
                nblk = 4 * i + 4
                for bi, sbk in enumerate(range(nblk - 1, -1, -1)):
                    diag = sbk >= 4 * i
                    j = sbk - 4 * i
                    lp, wt, tm = Lp(it % 2), Wt(it % 2), tmp(it % 2)
                    it += 1
                    psz = rb().v()
                    mm(psz, kT(sbk * 128, 128), qT(i * 512, 512), True, True)
                    act(E, psz, AF.Exp)
                    act(lp, E, AF.Ln, bias=1.0)
                    if diag:
                        tt(lp, lp, M(j), ALU.mult)
                    psc = rb().v()
                    mm(psc, TRI, lp, True, False)
                    mm(psc, kT(sbk * 128, 128), nq, False, True)
                    if bi == 0:
                        act(wt, psc, AF.Exp, scale=-1.0)
                    else:
                        tt(tm, psc, R, ALU.add)
                        act(wt, tm, AF.Exp, scale=-1.0)
                    if diag:
                        tt(wt, wt, M(j), ALU.mult)
                    mm(pso, vtok(sbk, 1), wt, bi == 0, bi == nblk - 1)
                    if bi < nblk - 1:
                        psr = rb().v()
                        mm(psr, ONES, lp, True, True)
                        if bi == 0:
                            cpy(R, psr)
                        else:
                            tt(R, psr, R, ALU.add)
                act(bv(R1_OFF + hh * SEQ + i * 512, 512), pso, AF.Copy)

        def out_proj():
            for g in range(2):
                w = next_w()
                wv = lambda k, c0, n: bv(w + k * 512 + c0, n)
                for cc in range(4):
                    c = g * 4 + cc
                    for t in range(4):
                        ps = rb().v()
                        for k in range(8):
                            mm(ps, wv(k, cc * 128, 128), bv(R2_OFF + k * SEQ + t * 512, 512), k == 0, k == 7)
                        tt(Xv(c, t * 512, 512), ps, Xv(c, t * 512, 512), ALU.add)

        def mlp():
            upT = lambda kk, c0, n: bv(R1_OFF + kk * SEQ + c0, n)
            sq = lambda i: fv(SCR + i * 1024, 512)
            it = 0
            for hf in range(2):
                for g in range(4):
                    w = next_w()
                    wv = lambda k, c0, n: bv(w + k * 512 + c0, n)
                    for jj in range(4):
                        for t in range(4):
                            ps = rb().v()
                            for k in range(8):
                                mm(ps, wv(k, jj * 128, 128), hT(k, t * 512, 512), k == 0, k == 7)
                            s_ = sq(it % 2)
                            it += 1
                            act(s_, ps, AF.Square)
                            stt(upT(g * 4 + jj, t * 512, 512), ps, 0.0, s_, ALU.is_gt, ALU.mult)
                for cg in range(4):
                    w = next_w()
                    wv = lambda k, c0, n: bv(w + k * 256 + c0, n)
                    for cc in range(2):
                        c = cg * 2 + cc
                        for t in range(4):
                            ps = rb().v()
                            for k in range(16):
                                mm(ps, wv(k, cc * 128, 128), upT(k, t * 512, 512), k == 0, k == 15)
                            tt(Xv(c, t * 512, 512), ps, Xv(c, t * 512, 512), ALU.add)

        class _Stop(Exception):
            pass

        def chk(i):
            if stop == i:
                whole = A.v()
                S.dma(SP, osem, dbg, whole.ap, reads=[whole])
                raise _Stop()

        try:
            for s in range(NSEQ):
                for k in range(8):
                    d = Xv(k, 0, SEQ)
                    S.dma(SP, xsem, d.ap, xin[s, :, k * SEQ:(k + 1) * SEQ], writes=[d])
                for l in range(NL):
                    rmsnorm(C_GMIX + l * 8, lambda k, t: hT(k, t * 512, 512))
                    chk(1)
                    for hh in range(4):
                        retention_head(hh)
                    chk(2)
                    gated_proj(R1_OFF, True)
                    chk(3)
                    mk = bv(SCR + 12800, 2048)
                    S.dma(POOL, csem, mk.ap, cmask, writes=[mk])
                    for hh in range(8):
                        sb_head(hh)
                    chk(4)
                    gated_proj(R1_OFF, False)
                    chk(5)
                    out_proj()
                    chk(6)
                    rmsnorm(C_GMLP + l * 8, lambda k, t: hT(k, t * 512, 512))
                    mlp()
                    chk(7)
                ost = lambda t, k: fv(HT_OFF + (t % 2) * 8192 + 2 * k * 512, 512)

                def store(t, s=s):
                    src = fv(HT_OFF + (t % 2) * 8192, 4096)
                    dst = out[s].rearrange("p (k t) -> p k t", k=8)[:, :, t * 512:(t + 1) * 512]
                    S.dma(SP, osem, dst, src.ap.rearrange("p (k t) -> p k t", k=8), reads=[src])

                rmsnorm(C_GFIN, lambda k, t: ost(t, k), after=store)
        except _Stop:
            pass
        stats = S.emit(out_sems=[osem])
    return nc, stats


def _pack(M):
    K, n = M.shape
    return np.ascontiguousarray(M.reshape(K // 128, 128, n).transpose(1, 0, 2).reshape(128, (K // 128) * n))


def _pad(a):
    o = np.zeros((128, GW), np.float32)
    o[:, :a.shape[1]] = a
    return o


def pack_weights(w_in, p_ret, p_sb, w_out, w_up, w_down, NL):
    wall = np.zeros((NL, NG, 128, GW), np.float32)
    OQ, OK_, OV, OG, SQ, SK, SV, GA, GB = 0, 512, 1024, 2048, 3072, 4096, 5120, 6144, 7168
    for l in range(NL):
        wi = w_in[l]
        g = 0
        for hh in range(4):
            q = wi[:, OQ + hh * 128:OQ + (hh + 1) * 128]
            k = wi[:, OK_ + hh * 128:OK_ + (hh + 1) * 128]
            sw = lambda m: np.concatenate([m[:, 64:], m[:, :64]], 1)
            wall[l, g] = _pack(np.concatenate([q, sw(q), k, sw(k)], 1)); g += 1
            v = wi[:, OV + hh * 256:OV + (hh + 1) * 256]
            gg = wi[:, OG + hh * 256:OG + (hh + 1) * 256]
            wall[l, g] = _pack(np.concatenate([v, gg], 1)); g += 1
        for gi in range(4):
            wall[l, g] = _pack(np.concatenate([p_ret[l][:, gi * 256:(gi + 1) * 256],
                                               wi[:, GA + gi * 256:GA + (gi + 1) * 256]], 1)); g += 1
        for hh in range(8):
            m = np.concatenate([wi[:, SQ + hh * 128:SQ + (hh + 1) * 128], wi[:, SK + hh * 128:SK + (hh + 1) * 128],
                                wi[:, SV + hh * 128:SV + (hh + 1) * 128]], 1)
            wall[l, g] = _pad(_pack(m)); g += 1
        for gi in range(4):
            wall[l, g] = _pack(np.concatenate([p_sb[l][:, gi * 256:(gi + 1) * 256],
                                               wi[:, GB + gi * 256:GB + (gi + 1) * 256]], 1)); g += 1
        for gi in range(2):
            wall[l, g] = _pack(w_out[l][:, gi * 512:(gi + 1) * 512]); g += 1
        for hf in range(2):
            for gi in range(4):
                wall[l, g] = _pack(w_up[l][:, hf * 2048 + gi * 512:hf * 2048 + (gi + 1) * 512]); g += 1
            for cg in range(4):
                wall[l, g] = _pack(w_down[l][hf * 2048:(hf + 1) * 2048, cg * 256:(cg + 1) * 256]); g += 1
        assert g == NG
    return wall


def const_tables():
    p = np.arange(128)
    half = 64
    inv_freq = (10000.0 ** (-(np.arange(half, dtype=np.float32)) / half)).astype(np.float32)
    pos = np.arange(SEQ, dtype=np.float32)
    ang = pos[None, :] * inv_freq[p % 64][:, None]
    cos = np.cos(ang).astype(np.float32)
    sin = np.sin(ang).astype(np.float32)
    sinS = np.where((p < 64)[:, None], -sin, sin).astype(np.float32)
    ctab = np.concatenate([cos, sinS], 1).astype(np.float32)
    idx = np.arange(128, dtype=np.float64)
    cdt = np.zeros((128, 4 * 512), np.float32)
    csm_dec = np.zeros((128, 8), np.float32)
    for h in range(4):
        lg = np.log1p(-(2.0 ** (-5.0 - h)))
        diff = idx[None, :] - idx[:, None]
        dt = np.where(diff >= 0, np.exp(lg * np.maximum(diff, 0.0)), 0.0) * (128.0 ** -0.5)
        cdt[:, h * 512:(h + 1) * 512] = np.tile(dt, (1, 4))
        csm_dec[:, h] = np.exp(lg * (127.0 - idx)) * (128.0 ** -0.5)
        csm_dec[:, 4 + h] = np.exp(lg * (idx + 1.0))
    cmask = np.zeros((128, 4 * 512), np.float32)
    tl = np.arange(512)
    for j in range(4):
        cmask[:, j * 512:(j + 1) * 512] = ((128 * j + p)[:, None] < tl[None, :]).astype(np.float32)
    tri = (p[:, None] >= p[None, :]).astype(np.float32)
    ctri = np.concatenate([tri, np.ones((128, 128), np.float32), np.eye(128, dtype=np.float32)], 1)
    return ctab, cdt, cmask, csm_dec, ctri


def make_inputs(x, w_in, p_ret, p_sb, w_out, w_up, w_down, g_mix, g_mlp, g_final, NL, NSEQ, ncores):
    x = np.asarray(x, np.float32)
    B = x.shape[0]
    xT = np.ascontiguousarray(x.reshape(B, SEQ, 8, 128).transpose(0, 3, 2, 1)).reshape(B, 128, 8 * SEQ)
    wall = pack_weights(np.asarray(w_in), np.asarray(p_ret), np.asarray(p_sb), np.asarray(w_out),
                        np.asarray(w_up), np.asarray(w_down), NL)
    ctab, cdt, cmask, dec, ctri = const_tables()
    csm = np.zeros((128, NSM), np.float32)
    gm = np.asarray(g_mix, np.float32).reshape(DEPTH, 8, 128)
    gl = np.asarray(g_mlp, np.float32).reshape(DEPTH, 8, 128)
    gf = np.asarray(g_final, np.float32).reshape(8, 128)
    for l in range(DEPTH):
        csm[:, C_GMIX + l * 8:C_GMIX + (l + 1) * 8] = gm[l].T
        csm[:, C_GMLP + l * 8:C_GMLP + (l + 1) * 8] = gl[l].T
    csm[:, C_GFIN:C_GFIN + 8] = gf.T
    csm[:, C_KDEC:C_KDEC + 8] = dec
    maps = []
    for c in range(ncores):
        maps.append({"xin": xT[c * NSEQ:(c + 1) * NSEQ], "wall": wall, "ctab": ctab, "cdt": cdt,
                     "cmask": cmask, "csm": csm, "ctri": ctri})
    return maps


def unpack_out(res, NSEQ):
    outs = []
    for r in res:
        o = np.asarray(r["out"]).reshape(NSEQ, 128, 8, SEQ)
        outs.append(o.transpose(0, 3, 2, 1).reshape(NSEQ, SEQ, D))
    return np.concatenate(outs, 0)


def kernel(x, w_in, p_ret, p_sb, w_out, w_up, w_down, g_mix, g_mlp, g_final):
    NSEQ = x.shape[0] // NCORES
    nc, _ = build(DEPTH, NSEQ)
    maps = make_inputs(x, w_in, p_ret, p_sb, w_out, w_up, w_down, g_mix, g_mlp, g_final, DEPTH, NSEQ, NCORES)
    res = run_bass_kernel_spmd(nc, maps, core_ids=list(range(NCORES)))
    return unpack_out(res.results, NSEQ).astype(np.float32)
```

```python
import contextlib
import numpy as np
import concourse.bass as bass
import concourse.mybir as mybir
from concourse.bass_utils import run_bass_kernel_spmd

F32 = mybir.dt.float32
BF16 = mybir.dt.bfloat16
ALU = mybir.AluOpType
AF = mybir.ActivationFunctionType
AX = mybir.AxisListType

PE, ACT, DVE, POOL, SP = "pe", "act", "dve", "pool", "sp"
SEM_LIMIT = 60000
DMA_LIMIT = 1800


class V:
    __slots__ = ("ap", "buf", "p0", "p1", "lo", "hi")

    def __init__(self, ap, buf, p0, p1, lo, hi):
        self.ap, self.buf, self.p0, self.p1, self.lo, self.hi = ap, buf, p0, p1, lo, hi

    def re(self, pattern, **kw):
        return V(self.ap.rearrange(pattern, **kw), self.buf, self.p0, self.p1, self.lo, self.hi)

    def with_ap(self, ap):
        return V(ap, self.buf, self.p0, self.p1, self.lo, self.hi)


class Buf:
    def __init__(self, name, handle, P, F, dtype, is_psum=False):
        self.name, self.h, self.P, self.F, self.dtype = name, handle, P, F, dtype
        self.is_psum = is_psum
        self.recs = []

    def v(self, lo=0, hi=None, p0=0, p1=None, f32=False):
        if hi is None:
            hi = self.F
        if p1 is None:
            p1 = self.P
        assert 0 <= lo < hi <= self.F and 0 <= p0 < p1 <= self.P, (self.name, lo, hi, p0, p1)
        ap = self.h[p0:p1, lo:hi]
        if f32:
            assert lo % 2 == 0 and hi % 2 == 0
            ap = ap.bitcast(F32)
        return V(ap, self, p0, p1, lo, hi)


class DmaSem:
    def __init__(self, sched, name):
        self.sched, self.name = sched, name
        self.hw = [sched._new_sem(name + "_0")]
        self.count = 0
        self.finals = []
        self.closed = 0
        self.consumers = []


class Op:
    __slots__ = ("eng", "fn", "deps", "dsem", "dsem_hw", "dsem_val", "signal", "sigval", "is_dma")

    def __init__(self, eng, fn, deps, dsem=None):
        self.eng, self.fn, self.deps, self.dsem = eng, fn, deps, dsem
        self.is_dma = dsem is not None
        self.signal = False
        self.sigval = None
        self.dsem_hw = None
        self.dsem_val = None


class Sched:
    def __init__(self, nc, stack):
        self.nc, self.stack = nc, stack
        self.ops = []
        self.bufs = []
        self.nsem = 0
        self.final_waits = []

    def _new_sem(self, name):
        self.nsem += 1
        return self.stack.enter_context(self.nc.semaphore(name))

    def sbuf(self, name, P, F, dtype):
        h = self.stack.enter_context(self.nc.sbuf_tensor(name, [P, F], dtype))
        b = Buf(name, h, P, F, dtype)
        self.bufs.append(b)
        return b

    def psum(self, name, P, F, dtype):
        h = self.stack.enter_context(self.nc.psum_tensor(name, [P, F], dtype))
        b = Buf(name, h, P, F, dtype, is_psum=True)
        self.bufs.append(b)
        return b

    def dma_sem(self, name):
        return DmaSem(self, name)

    def _access(self, v, n, eng, is_write, is_dma):
        deps = set()
        recs = v.buf.recs
        keep = []
        psum = v.buf.is_psum
        if psum:
            v = V(v.ap, v.buf, 0, v.buf.P, 0, v.buf.F)
        for r in recs:
            ov = not (r[1] <= v.p0 or v.p1 <= r[0] or r[3] <= v.lo or v.hi <= r[2])
            if ov and (r[5] or is_write or (psum and r[6] != eng)) and r[4] != n:
                deps.add(r[4])
            covered = ov and v.p0 <= r[0] and r[1] <= v.p1 and v.lo <= r[2] and r[3] <= v.hi
            if covered and r[4] != n:
                if is_write:
                    continue
                if (not r[5]) and (not is_dma) and r[6] == eng:
                    continue
            keep.append(r)
        keep.append([v.p0, v.p1, v.lo, v.hi, n, is_write, None if is_dma else eng])
        v.buf.recs = keep
        return deps

    def op(self, eng, fn, reads=(), writes=(), dsem=None):
        n = len(self.ops)
        is_dma = dsem is not None
        deps = set()
        for v in reads:
            deps |= self._access(v, n, eng, False, is_dma)
        for v in writes:
            deps |= self._access(v, n, eng, True, is_dma)
        deps.discard(n)
        o = Op(eng, fn, None, dsem)
        if is_dma:
            if dsem.consumers:
                deps |= set(dsem.consumers)
                dsem.consumers = []
                if dsem.count > DMA_LIMIT:
                    dsem.finals.append(dsem.count * 16)
                    dsem.hw.append(self._new_sem(dsem.name + "_%d" % len(dsem.hw)))
                    dsem.count = 0
                dsem.closed = dsem.count
            dsem.count += 1
            o.dsem_hw = len(dsem.hw) - 1
            o.dsem_val = dsem.count * 16
        final = []
        for d in deps:
            p = self.ops[d]
            if p.is_dma:
                if p.dsem_hw != len(p.dsem.hw) - 1:
                    final.append(("d", p.dsem, p.dsem_hw, p.dsem.finals[p.dsem_hw]))
                    continue
                if p.dsem is dsem:
                    if p.dsem_val > dsem.closed * 16:
                        final.append(("d", p.dsem, p.dsem_hw, (dsem.count - 1) * 16))
                    continue
                final.append(("d", p.dsem, p.dsem_hw, p.dsem.count * 16))
                p.dsem.consumers.append(n)
            else:
                if p.eng == PE and eng == PE:
                    continue
                p.signal = True
                final.append(("e", d))
        o.deps = final
        self.ops.append(o)
        return n

    def pe(self, fn, reads=(), writes=()):
        return self.op(PE, fn, reads, writes)

    def act(self, fn, reads=(), writes=()):
        return self.op(ACT, fn, reads, writes)

    def dve(self, fn, reads=(), writes=()):
        return self.op(DVE, fn, reads, writes)

    def pool(self, fn, reads=(), writes=()):
        return self.op(POOL, fn, reads, writes)

    def dma(self, eng, dsem, out, in_, reads=(), writes=()):
        return self.op(eng, lambda e: e.dma_start(out=out, in_=in_), reads, writes, dsem=dsem)

    def emit(self, out_sems=()):
        nc = self.nc
        engs = [PE, ACT, DVE, POOL, SP]
        ops = self.ops
        per_eng = {e: [] for e in engs}
        pos = {}
        for i, o in enumerate(ops):
            pos[i] = len(per_eng[o.eng])
            per_eng[o.eng].append(i)

        plan = {}
        for e in engs:
            wp = {}
            for i in per_eng[e]:
                best = {}
                for d in ops[i].deps:
                    if d[0] == "e":
                        p = ops[d[1]]
                        if p.eng not in best or pos[d[1]] > pos[best[p.eng]]:
                            best[p.eng] = d[1]
                lst = []
                for pe_, pi in best.items():
                    if wp.get(pe_, -1) >= pos[pi]:
                        continue
                    wp[pe_] = pos[pi]
                    lst.append(pi)
                plan[i] = lst
        for o in ops:
            o.signal = False
        for i, lst in plan.items():
            for pi in lst:
                ops[pi].signal = True
        counts = {e: 0 for e in engs}
        for o in ops:
            if o.signal and not o.is_dma:
                counts[o.eng] += 1
                o.sigval = counts[o.eng]
        esems = {}
        for e in engs:
            nchunks = (counts[e] + SEM_LIMIT - 1) // SEM_LIMIT
            esems[e] = [self._new_sem("sig_%s_%d" % (e, k)) for k in range(max(nchunks, 1))]
        stats = {e: [len(per_eng[e]), 0, counts[e]] for e in engs}

        def replay(eng_name, eobj):
            waited = {}
            for i in per_eng[eng_name]:
                o = ops[i]
                need = {}
                for d in o.deps:
                    if d[0] == "d":
                        key = ("d", id(d[1]), d[2])
                        sem = d[1].hw[d[2]]
                        val = d[3]
                        if waited.get(key, 0) >= val:
                            continue
                        if key not in need or need[key][1] < val:
                            need[key] = (sem, val)
                for pi in plan[i]:
                    p = ops[pi]
                    ch = (p.sigval - 1) // SEM_LIMIT
                    need[("e", p.eng, ch, pi)] = (esems[p.eng][ch], (p.sigval - 1) % SEM_LIMIT + 1)
                for key, (sem, val) in need.items():
                    eobj.wait_ge(sem, val)
                    if key[0] == "d":
                        waited[key] = val
                    stats[eng_name][1] += 1
                ins = o.fn(eobj)
                if o.is_dma:
                    ins.then_inc(o.dsem.hw[o.dsem_hw], 16)
                elif o.signal:
                    ch = (o.sigval - 1) // SEM_LIMIT
                    ins.then_inc(esems[o.eng][ch], 1)
            if eng_name == SP:
                for ds in out_sems:
                    eobj.wait_ge(ds.hw[-1], ds.count * 16)

        with nc.Block() as block:
            @block.tensor
            def _(e):
                replay(PE, e)

            @block.scalar
            def _(e):
                replay(ACT, e)

            @block.vector
            def _(e):
                replay(DVE, e)

            @block.gpsimd
            def _(e):
                replay(POOL, e)

            @block.sync
            def _(e):
                replay(SP, e)
        return stats


D = 1024
SEQ = 2048
DEPTH = 4
NCORES = 8
NG = 42
GW = 4096
EPS = 1e-6
GN_EPS = 1e-5
T = 512
PARTIAL = True
GAMMAS = [1.0 - 2.0 ** (-5.0 - h) for h in range(4)]

X_OFF = 0
HT_OFF = 32768
R1_OFF = 49152
R2_OFF = 65536
W_OFF = [81920, 86016]
SCR = 90112
CST = 104960
ARENA = 105984
C_GMIX, C_GMLP, C_GFIN, C_KDEC, C_QDEC, NSM = 0, 32, 64, 72, 76, 80
TRI_OFF, ONES_OFF, ID_OFF, NTRI_OFF = CST + 160, CST + 288, CST + 416, CST + 544


def build(NL, NSEQ, stop=None):
    nc = bass.Bass("TRN2", target_bir_lowering=False)
    xin = nc.dram_tensor("xin", [NSEQ, 128, 8 * SEQ], F32, kind="ExternalInput").ap()
    wall = nc.dram_tensor("wall", [NL, NG, 128, GW], F32, kind="ExternalInput").ap()
    ctab = nc.dram_tensor("ctab", [128, 2 * SEQ], F32, kind="ExternalInput").ap()
    cdt = nc.dram_tensor("cdt", [128, 4 * 512], F32, kind="ExternalInput").ap()
    cmask = nc.dram_tensor("cmask", [128, 4 * 512], F32, kind="ExternalInput").ap()
    csm = nc.dram_tensor("csm", [128, NSM], F32, kind="ExternalInput").ap()
    ctri = nc.dram_tensor("ctri", [128, 4 * 128], F32, kind="ExternalInput").ap()
    out = nc.dram_tensor("out", [NSEQ, 128, 8 * SEQ], F32, kind="ExternalOutput").ap()
    dbg = nc.dram_tensor("dbg", [128, ARENA], BF16, kind="ExternalOutput").ap() if stop is not None else None

    with contextlib.ExitStack() as st:
        S = Sched(nc, st)
        A = S.sbuf("arena", 128, ARENA, BF16)
        PB = [S.psum("pb%d" % i, 128, 512, F32) for i in range(7)]
        PT = S.psum("pt", 128, 1024, BF16)
        rot = [0]

        def rb():
            rot[0] = (rot[0] + 1) % 5
            return PB[rot[0]]

        wsem = [S.dma_sem("w0"), S.dma_sem("w1")]
        xsem = S.dma_sem("x")
        csem = S.dma_sem("c")
        cpsem = S.dma_sem("cp")
        tsem = [S.dma_sem("t0"), S.dma_sem("t1")]
        osem = S.dma_sem("o")

        def bv(off, n):
            return A.v(off, off + n)

        def fv(off, n):
            return A.v(off, off + 2 * n, f32=True)

        def Xv(k, c0, n):
            return fv(X_OFF + 2 * (k * SEQ + c0), n)

        def hT(k, c0, n):
            return bv(HT_OFF + k * SEQ + c0, n)

        def cs(col):
            return fv(CST + 2 * col, 1)

        TRI = bv(TRI_OFF, 128)
        ONES = bv(ONES_OFF, 128)
        IDENT = bv(ID_OFF, 128)
        NTRI = bv(NTRI_OFF, 128)

        def mm(ps, lhsT, rhs, start, stop):
            S.pe(lambda e: e.matmul(ps.ap, lhsT=lhsT.ap, rhs=rhs.ap, start=start, stop=stop),
                 reads=[lhsT, rhs], writes=[ps])

        def tr(ps, in_):
            S.pe(lambda e: e.transpose(ps.ap, in_.ap, IDENT.ap), reads=[in_, IDENT], writes=[ps])

        def act(o, i, func, scale=1.0, bias=0.0):
            reads = [i]
            sc, bi = scale, bias
            if isinstance(scale, V):
                reads.append(scale)
                sc = scale.ap
            if isinstance(bias, V):
                reads.append(bias)
                bi = bias.ap
            S.act(lambda e: e.activation(out=o.ap, in_=i.ap, func=func, scale=sc, bias=bi), reads=reads, writes=[o])

        def tt(o, a, b, op):
            S.dve(lambda e: e.tensor_tensor(out=o.ap, in0=a.ap, in1=b.ap, op=op), reads=[a, b], writes=[o])

        def ts(o, a, s1, s2, op0, op1=None):
            reads = [a]
            v1, v2 = s1, s2
            if isinstance(s1, V):
                reads.append(s1)
                v1 = s1.ap
            if isinstance(s2, V):
                reads.append(s2)
                v2 = s2.ap
            if op1 is None:
                S.dve(lambda e: e.tensor_scalar(out=o.ap, in0=a.ap, scalar1=v1, scalar2=None, op0=op0),
                      reads=reads, writes=[o])
            else:
                S.dve(lambda e: e.tensor_scalar(out=o.ap, in0=a.ap, scalar1=v1, scalar2=v2, op0=op0, op1=op1),
                      reads=reads, writes=[o])

        def stt(o, a, sc, b, op0, op1):
            reads = [a, b]
            v = sc
            if isinstance(sc, V):
                reads.append(sc)
                v = sc.ap
            S.dve(lambda e: e.scalar_tensor_tensor(out=o.ap, in0=a.ap, scalar=v, in1=b.ap, op0=op0, op1=op1),
                  reads=reads, writes=[o])

        def cpy(o, i):
            S.dve(lambda e: e.tensor_copy(out=o.ap, in_=i.ap), reads=[i], writes=[o])

        def sigmoid_to(o, ps, t1, t2):
            act(t1, ps, AF.Exp, scale=-1.0)
            act(t2, t1, AF.Ln, bias=1.0)
            act(o, t2, AF.Exp, scale=-1.0)

        wstate = {"next": 0, "list": []}
        for l in range(NL):
            for g in range(NG):
                wstate["list"].append((l, g))
        wstate["list"] = wstate["list"] * NSEQ
        nW = len(wstate["list"])
        wcnt = [0]

        def issue_w(i):
            l, g = wstate["list"][i]
            sl = i % 2
            dst = bv(W_OFF[sl], GW)
            S.dma(POOL, wsem[sl], dst.ap, wall[l, g], writes=[dst])

        def next_w():
            i = wcnt[0]
            if i == 0:
                issue_w(0)
            if i + 1 < nW:
                issue_w(i + 1)
            wcnt[0] += 1
            return W_OFF[i % 2]

        d0 = fv(CST, NSM)
        S.dma(SP, csem, d0.ap, csm, writes=[d0])
        d1 = bv(TRI_OFF, 512)
        S.dma(POOL, cpsem, d1.ap, ctri, writes=[d1])

        def rmsnorm(gbase, dst_fn, after=None):
            sqb = lambda k: bv(SCR + k * 512, 512)
            lnt = fv(SCR + 4096, 512)
            rstd = fv(SCR + 5120, 512)
            for t in range(4):
                c0 = t * 512
                for k in range(8):
                    act(sqb(k), Xv(k, c0, 512), AF.Square)
                ps = rb().v()
                for k in range(8):
                    mm(ps, ONES, sqb(k), k == 0, k == 7)
                act(lnt, ps, AF.Ln, scale=1.0 / D, bias=EPS)
                act(rstd, lnt, AF.Exp, scale=-0.5)
                for k in range(8):
                    stt(dst_fn(k, t), Xv(k, c0, 512), cs(gbase + k), rstd, ALU.mult, ALU.mult)
                if after is not None:
                    after(t)

        def retention_head(hh):
            qT = lambda c0, n: bv(R2_OFF + c0, n)
            kT = lambda c0, n: bv(R2_OFF + 2048 + c0, n)
            ktok = lambda n0, nn: bv(R2_OFF + 4096 + n0 * 128, nn * 128)
            vtok = lambda n0, nn: bv(R2_OFF + 6144 + n0 * 256, nn * 256)
            sg = lambda n0, nn: bv(R2_OFF + 10240 + n0 * 256, nn * 256)
            y4 = lambda j0, nj: fv(R2_OFF + 14336 + 2 * j0 * 256, nj * 256)
            cosb = lambda i: fv(SCR + i * 1024, 512)
            sinb = lambda i: fv(SCR + 2048 + i * 1024, 512)
            DT = fv(SCR + 4096, 512)
            tmp1 = fv(SCR + 5120, 512)
            tmp2 = fv(SCR + 6144, 512)
            Sf = lambda i: fv(SCR + 7168 + i * 512, 256)
            Sb = lambda n: bv(SCR + 8192 + n * 256, 256)
            scT = lambda i: bv(SCR + 12288 + i * 512, 512)
            rtok = bv(SCR + 13312, 1024)
            bnst = lambda j: fv(SCR + 14336 + j * 12, 6)
            mv = lambda j: fv(SCR + 14336 + 48 + j * 4, 2)
            mvall = fv(SCR + 14336 + 48, 8)
            lnv = fv(SCR + 14336 + 64, 4)
            rs4 = fv(SCR + 14336 + 72, 4)
            kdec = cs(C_KDEC + hh)
            qdec = cs(C_QDEC + hh)
            gC = GAMMAS[hh] ** 128

            S.dma(SP, csem, DT.ap, cdt[:, hh * 512:(hh + 1) * 512], writes=[DT])
            w = next_w()
            wv = lambda k, c0, n: bv(w + k * 512 + c0, n)
            for t in range(4):
                c0 = t * 512
                cb, sb_ = cosb(t % 2), sinb(t % 2)
                S.dma(SP, tsem[t % 2], cb.ap, ctab[:, c0:c0 + 512], writes=[cb])
                S.dma(SP, tsem[t % 2], sb_.ap, ctab[:, SEQ + c0:SEQ + c0 + 512], writes=[sb_])
                for dst, col in ((qT, 0), (kT, 256)):
                    ps1 = rb().v()
                    for k in range(8):
                        mm(ps1, wv(k, col, 128), hT(k, c0, 512), k == 0, k == 7)
                    ps2 = rb().v()
                    for k in range(8):
                        mm(ps2, wv(k, col + 128, 128), hT(k, c0, 512), k == 0, k == 7)
                    tt(tmp1, ps1, cb, ALU.mult)
                    tt(tmp2, ps2, sb_, ALU.mult)
                    tt(dst(c0, 512), tmp1, tmp2, ALU.add)
            w = next_w()
            for t in range(4):
                for pr in range(2):
                    n0 = t * 4 + pr * 2
                    psv = rb()
                    for j in range(2):
                        o = psv.v(j * 256, (j + 1) * 256)
                        for k in range(8):
                            mm(o, hT(k, (n0 + j) * 128, 128), wv(k, 0, 256), k == 0, k == 7)
                    act(vtok(n0, 2), psv.v(), AF.Copy)
                    psg = rb()
                    for j in range(2):
                        o = psg.v(j * 256, (j + 1) * 256)
                        for k in range(8):
                            mm(o, hT(k, (n0 + j) * 128, 128), wv(k, 256, 256), k == 0, k == 7)
                    sigmoid_to(tmp1, psg.v(), tmp1, tmp2)
                    tt(sg(n0, 2), psg.v(), tmp1, ALU.mult)
            for t in range(4):
                for j in range(4):
                    tr(PT.v(j * 128, (j + 1) * 128), kT((t * 4 + j) * 128, 128))
                act(ktok(t * 4, 4), PT.v(0, 512), AF.Copy, scale=kdec)
            S.dve(lambda e: e.memset(Sb(0).ap, 0.0), writes=[Sb(0)])
            for n in range(15):
                ps = rb().v(0, 256)
                mm(ps, ktok(n, 1), vtok(n, 1), True, True)
                if n == 0:
                    cpy(Sf(0), ps)
                else:
                    stt(Sf(n % 2), Sf((n - 1) % 2), gC, ps, ALU.mult, ALU.add)
                act(Sb(n + 1), Sf(n % 2), AF.Copy)
            for t in range(4):
                pss = rb()
                for j in range(4):
                    n = t * 4 + j
                    mm(pss.v(j * 128, (j + 1) * 128), kT(n * 128, 128), qT(n * 128, 128), True, True)
                sc = scT(t % 2)
                tt(sc, pss.v(), DT, ALU.mult)
                for pr in range(2):
                    pso, psx = rb(), rb()
                    for j in range(2):
                        jj = pr * 2 + j
                        n = t * 4 + jj
                        mm(pso.v(j * 256, (j + 1) * 256), bv(SCR + 12288 + (t % 2) * 512 + jj * 128, 128), vtok(n, 1), True, True)
                        mm(psx.v(j * 256, (j + 1) * 256), qT(n * 128, 128), Sb(n), True, True)
                    act(tmp1, psx.v(), AF.Copy, scale=qdec)
                    tt(y4(pr * 2, 2), pso.v(), tmp1, ALU.add)
                for j in range(4):
                    S.dve(lambda e, j=j: e.bn_stats(out=bnst(j).ap, in_=y4(j, 1).ap), reads=[y4(j, 1)], writes=[bnst(j)])
                    S.dve(lambda e, j=j: e.bn_aggr(out=mv(j).ap, in_=bnst(j).ap), reads=[bnst(j)], writes=[mv(j)])
                mvar = mvall.with_ap(mvall.ap.rearrange("p (j c) -> p j c", c=2)[:, :, 1])
                act(lnv, mvar, AF.Ln, bias=GN_EPS)
                act(rs4, lnv, AF.Exp, scale=-0.5)
                for j in range(4):
                    n = t * 4 + j
                    tj = fv(SCR + 5120 + (j % 2) * 1024, 256)
                    ts(tj, y4(j, 1), fv(SCR + 14336 + 48 + j * 4, 1), fv(SCR + 14336 + 72 + j * 2, 1), ALU.subtract, ALU.mult)
                    tt(bv(SCR + 13312 + j * 256, 256), tj, sg(n, 1), ALU.mult)
                for e2 in range(2):
                    for j in range(4):
                        tr(PT.v((e2 * 4 + j) * 128, (e2 * 4 + j + 1) * 128), bv(SCR + 13312 + j * 256 + e2 * 128, 128))
                for e2 in range(2):
                    act(bv(R1_OFF + (2 * hh + e2) * SEQ + t * 512, 512), PT.v(e2 * 512, (e2 + 1) * 512), AF.Copy)

        def gated_proj(src_off, first):
            e1 = fv(SCR, 512)
            e2 = fv(SCR + 1024, 512)
            sgm = lambda i: fv(SCR + 2048 + i * 1024, 512)
            tb = fv(SCR + 4096, 512)
            for g in range(4):
                w = next_w()
                wv = lambda k, c0, n: bv(w + k * 512 + c0, n)
                for cc in range(2):
                    c = g * 2 + cc
                    for t in range(4):
                        psa, psg = rb().v(), rb().v()
                        for k in range(8):
                            mm(psa, wv(k, cc * 128, 128), bv(src_off + k * SEQ + t * 512, 512), k == 0, k == 7)
                        for k in range(8):
                            mm(psg, wv(k, 256 + cc * 128, 128), hT(k, t * 512, 512), k == 0, k == 7)
                        sm = sgm(t % 2)
                        sigmoid_to(sm, psg, e1, e2)
                        ga = bv(R2_OFF + c * SEQ + t * 512, 512)
                        if first:
                            tt(ga, psa, sm, ALU.mult)
                        else:
                            tt(tb, psa, sm, ALU.mult)
                            tt(ga, ga, tb, ALU.add)

        def sb_head(hh):
            qT = lambda c0, n: bv(SCR + c0, n)
            kT = lambda c0, n: bv(SCR + 2048 + c0, n)
            vtok = lambda n0, nn: bv(SCR + 4096 + n0 * 128, nn * 128)
            E = lambda lo: fv(SCR + 6144 + 2 * lo, 512 - lo)
            Lp = lambda i, lo: bv(SCR + 7168 + i * 512 + lo, 512 - lo)
            tmp = lambda lo: fv(SCR + 8704 + 2 * lo, 512 - lo)
            Wt = lambda i, lo: bv(SCR + 9728 + i * 512 + lo, 512 - lo)
            R = lambda i, lo: fv(SCR + 10752 + i * 1024 + 2 * lo, 512 - lo)
            M = lambda j, lo: bv(SCR + 12800 + j * 512 + lo, 512 - lo)
            scale = 128.0 ** -0.5
            w = next_w()
            wv = lambda k, c0, n: bv(w + k * 384 + c0, n)
            for t in range(4):
                psq = rb().v()
                for k in range(8):
                    mm(psq, wv(k, 0, 128), hT(k, t * 512, 512), k == 0, k == 7)
                act(qT(t * 512, 512), psq, AF.Copy, scale=scale)
                psk = rb().v()
                for k in range(8):
                    mm(psk, wv(k, 128, 128), hT(k, t * 512, 512), k == 0, k == 7)
                cpy(kT(t * 512, 512), psk)
                psv = rb()
                for j in range(4):
                    o = psv.v(j * 128, (j + 1) * 128)
                    for k in range(8):
                        mm(o, hT(k, (t * 4 + j) * 128, 128), wv(k, 256, 128), k == 0, k == 7)
                cpy(vtok(t * 4, 4), psv.v())
            blocks = []
            for i in range(4):
                nblk = 4 * i + 4
                for bi, sbk in enumerate(range(nblk - 1, -1, -1)):
                    j = sbk - 4 * i
                    lo = (128 * j if j > 0 else 0) if PARTIAL else 0
                    blocks.append(dict(i=i, bi=bi, sbk=sbk, j=j, lo=lo, diag=(j >= 0), last=(bi == nblk - 1), n=len(blocks)))

            def stage1(b):
                lo, i = b["lo"], b["i"]
                psz = rb()
                b["lp"] = Lp(b["n"] % 3, lo)
                mm(psz.v(lo, 512), kT(b["sbk"] * 128, 128), qT(i * 512 + lo, 512 - lo), True, True)
                act(E(lo), psz.v(lo, 512), AF.Exp)
                act(b["lp"], E(lo), AF.Ln, bias=1.0)
                if b["diag"]:
                    tt(b["lp"], b["lp"], M(b["j"], lo), ALU.mult)

            def stage2(b):
                lo, i = b["lo"], b["i"]
                if b["bi"] == 0:
                    rr = R(i % 2, 0)
                    S.dve(lambda e: e.memset(rr.ap, 0.0), writes=[rr])
                psc = rb()
                mm(psc.v(lo, 512), NTRI, b["lp"], True, False)
                mm(psc.v(lo, 512), kT(b["sbk"] * 128, 128), qT(i * 512 + lo, 512 - lo), False, True)
                if not b["last"]:
                    psr = rb()
                    mm(psr.v(lo, 512), ONES, b["lp"], True, True)
                b["wt"] = Wt(b["n"] % 2, lo)
                if b["bi"] == 0:
                    act(b["wt"], psc.v(lo, 512), AF.Exp)
                else:
                    tt(tmp(lo), psc.v(lo, 512), R(i % 2, lo), ALU.subtract)
                    act(b["wt"], tmp(lo), AF.Exp)
                if not b["last"]:
                    tt(R(i % 2, lo), psr.v(lo, 512), R(i % 2, lo), ALU.add)
                if b["diag"]:
                    tt(b["wt"], b["wt"], M(b["j"], lo), ALU.mult)

            def stage3(b):
                lo, i = b["lo"], b["i"]
                pso = PB[5 + (i % 2)]
                mm(pso.v(lo, 512), vtok(b["sbk"], 1), b["wt"], b["bi"] == 0, b["last"])
                if b["last"]:
                    act(bv(R1_OFF + hh * SEQ + i * 512, 512), pso.v(), AF.Copy)

            nb = len(blocks)
            for s_ in range(nb + 2):
                if s_ < nb:
                    stage1(blocks[s_])
                if 0 <= s_ - 1 < nb:
                    stage2(blocks[s_ - 1])
                if 0 <= s_ - 2 < nb:
                    stage3(blocks[s_ - 2])

        def out_proj():
            for g in range(2):
                w = next_w()
                wv = lambda k, c0, n: bv(w + k * 512 + c0, n)
                for cc in range(4):
                    c = g * 4 + cc
                    for t in range(4):
                        ps = rb().v()
                        for k in range(8):
                            mm(ps, wv(k, cc * 128, 128), bv(R2_OFF + k * SEQ + t * 512, 512), k == 0, k == 7)
                        tt(Xv(c, t * 512, 512), ps, Xv(c, t * 512, 512), ALU.add)

        def mlp():
            upT = lambda kk, c0, n: bv(R1_OFF + kk * SEQ + c0, n)
            sq = lambda i: fv(SCR + i * 1024, 512)
            it = 0
            for hf in range(2):
                for g in range(4):
                    w = next_w()
                    wv = lambda k, c0, n: bv(w + k * 512 + c0, n)
                    for jj in range(4):
                        for t in range(4):
                            ps = rb().v()
                            for k in range(8):
                                mm(ps, wv(k, jj * 128, 128), hT(k, t * 512, 512), k == 0, k == 7)
                            s_ = sq(it % 2)
                            it += 1
                            act(s_, ps, AF.Square)
                            stt(upT(g * 4 + jj, t * 512, 512), ps, 0.0, s_, ALU.is_gt, ALU.mult)
                for cg in range(4):
                    w = next_w()
                    wv = lambda k, c0, n: bv(w + k * 256 + c0, n)
                    for cc in range(2):
                        c = cg * 2 + cc
                        for t in range(4):
                            ps = rb().v()
                            for k in range(16):
                                mm(ps, wv(k, cc * 128, 128), upT(k, t * 512, 512), k == 0, k == 15)
                            tt(Xv(c, t * 512, 512), ps, Xv(c, t * 512, 512), ALU.add)

        class _Stop(Exception):
            pass

        def chk(i):
            if stop == i:
                whole = A.v()
                S.dma(SP, osem, dbg, whole.ap, reads=[whole])
                raise _Stop()

        try:
            for s in range(NSEQ):
                for k in range(8):
                    d = Xv(k, 0, SEQ)
                    S.dma(SP, xsem, d.ap, xin[s, :, k * SEQ:(k + 1) * SEQ], writes=[d])
                for l in range(NL):
                    rmsnorm(C_GMIX + l * 8, lambda k, t: hT(k, t * 512, 512))
                    chk(1)
                    for hh in range(4):
                        retention_head(hh)
                    chk(2)
                    gated_proj(R1_OFF, True)
                    chk(3)
                    mk = bv(SCR + 12800, 2048)
                    S.dma(POOL, cpsem, mk.ap, cmask, writes=[mk])
                    for hh in range(8):
                        sb_head(hh)
                    chk(4)
                    gated_proj(R1_OFF, False)
                    chk(5)
                    out_proj()
                    chk(6)
                    rmsnorm(C_GMLP + l * 8, lambda k, t: hT(k, t * 512, 512))
                    mlp()
                    chk(7)
                ost = lambda t, k: fv(HT_OFF + (t % 2) * 8192 + 2 * k * 512, 512)

                def store(t, s=s):
                    src = fv(HT_OFF + (t % 2) * 8192, 4096)
                    dst = out[s].rearrange("p (k t) -> p k t", k=8)[:, :, t * 512:(t + 1) * 512]
                    S.dma(SP, osem, dst, src.ap.rearrange("p (k t) -> p k t", k=8), reads=[src])

                rmsnorm(C_GFIN, lambda k, t: ost(t, k), after=store)
        except _Stop:
            pass
        stats = S.emit(out_sems=[osem])
    return nc, stats


def _pack(M):
    K, n = M.shape
    return np.ascontiguousarray(M.reshape(K // 128, 128, n).transpose(1, 0, 2).reshape(128, (K // 128) * n))


def _pad(a):
    o = np.zeros((128, GW), np.float32)
    o[:, :a.shape[1]] = a
    return o


def pack_weights(w_in, p_ret, p_sb, w_out, w_up, w_down, NL):
    wall = np.zeros((NL, NG, 128, GW), np.float32)
    OQ, OK_, OV, OG, SQ, SK, SV, GA, GB = 0, 512, 1024, 2048, 3072, 4096, 5120, 6144, 7168
    for l in range(NL):
        wi = w_in[l]
        g = 0
        for hh in range(4):
            q = wi[:, OQ + hh * 128:OQ + (hh + 1) * 128]
            k = wi[:, OK_ + hh * 128:OK_ + (hh + 1) * 128]
            sw = lambda m: np.concatenate([m[:, 64:], m[:, :64]], 1)
            wall[l, g] = _pack(np.concatenate([q, sw(q), k, sw(k)], 1)); g += 1
            v = wi[:, OV + hh * 256:OV + (hh + 1) * 256]
            gg = wi[:, OG + hh * 256:OG + (hh + 1) * 256]
            wall[l, g] = _pack(np.concatenate([v, gg], 1)); g += 1
        for gi in range(4):
            wall[l, g] = _pack(np.concatenate([p_ret[l][:, gi * 256:(gi + 1) * 256],
                                               wi[:, GA + gi * 256:GA + (gi + 1) * 256]], 1)); g += 1
        for hh in range(8):
            m = np.concatenate([wi[:, SQ + hh * 128:SQ + (hh + 1) * 128], wi[:, SK + hh * 128:SK + (hh + 1) * 128],
                                wi[:, SV + hh * 128:SV + (hh + 1) * 128]], 1)
            wall[l, g] = _pad(_pack(m)); g += 1
        for gi in range(4):
            wall[l, g] = _pack(np.concatenate([p_sb[l][:, gi * 256:(gi + 1) * 256],
                                               wi[:, GB + gi * 256:GB + (gi + 1) * 256]], 1)); g += 1
        for gi in range(2):
            wall[l, g] = _pack(w_out[l][:, gi * 512:(gi + 1) * 512]); g += 1
        for hf in range(2):
            for gi in range(4):
                wall[l, g] = _pack(w_up[l][:, hf * 2048 + gi * 512:hf * 2048 + (gi + 1) * 512]); g += 1
            for cg in range(4):
                wall[l, g] = _pack(w_down[l][hf * 2048:(hf + 1) * 2048, cg * 256:(cg + 1) * 256]); g += 1
        assert g == NG
    return wall


def const_tables():
    p = np.arange(128)
    half = 64
    inv_freq = (10000.0 ** (-(np.arange(half, dtype=np.float32)) / half)).astype(np.float32)
    pos = np.arange(SEQ, dtype=np.float32)
    ang = pos[None, :] * inv_freq[p % 64][:, None]
    cos = np.cos(ang).astype(np.float32)
    sin = np.sin(ang).astype(np.float32)
    sinS = np.where((p < 64)[:, None], -sin, sin).astype(np.float32)
    ctab = np.concatenate([cos, sinS], 1).astype(np.float32)
    idx = np.arange(128, dtype=np.float64)
    cdt = np.zeros((128, 4 * 512), np.float32)
    csm_dec = np.zeros((128, 8), np.float32)
    for h in range(4):
        lg = np.log1p(-(2.0 ** (-5.0 - h)))
        diff = idx[None, :] - idx[:, None]
        dt = np.where(diff >= 0, np.exp(lg * np.maximum(diff, 0.0)), 0.0) * (128.0 ** -0.5)
        cdt[:, h * 512:(h + 1) * 512] = np.tile(dt, (1, 4))
        csm_dec[:, h] = np.exp(lg * (127.0 - idx)) * (128.0 ** -0.5)
        csm_dec[:, 4 + h] = np.exp(lg * (idx + 1.0))
    cmask = np.zeros((128, 4 * 512), np.float32)
    tl = np.arange(512)
    for j in range(4):
        cmask[:, j * 512:(j + 1) * 512] = ((128 * j + p)[:, None] < tl[None, :]).astype(np.float32)
    tri = (p[:, None] >= p[None, :]).astype(np.float32)
    ctri = np.concatenate([tri, np.ones((128, 128), np.float32), np.eye(128, dtype=np.float32), -tri], 1)
    return ctab, cdt, cmask, csm_dec, ctri


def make_inputs(x, w_in, p_ret, p_sb, w_out, w_up, w_down, g_mix, g_mlp, g_final, NL, NSEQ, ncores):
    x = np.asarray(x, np.float32)
    B = x.shape[0]
    xT = np.ascontiguousarray(x.reshape(B, SEQ, 8, 128).transpose(0, 3, 2, 1)).reshape(B, 128, 8 * SEQ)
    wall = pack_weights(np.asarray(w_in), np.asarray(p_ret), np.asarray(p_sb), np.asarray(w_out),
                        np.asarray(w_up), np.asarray(w_down), NL)
    ctab, cdt, cmask, dec, ctri = const_tables()
    csm = np.zeros((128, NSM), np.float32)
    gm = np.asarray(g_mix, np.float32).reshape(DEPTH, 8, 128)
    gl = np.asarray(g_mlp, np.float32).reshape(DEPTH, 8, 128)
    gf = np.asarray(g_final, np.float32).reshape(8, 128)
    for l in range(DEPTH):
        csm[:, C_GMIX + l * 8:C_GMIX + (l + 1) * 8] = gm[l].T
        csm[:, C_GMLP + l * 8:C_GMLP + (l + 1) * 8] = gl[l].T
    csm[:, C_GFIN:C_GFIN + 8] = gf.T
    csm[:, C_KDEC:C_KDEC + 8] = dec
    maps = []
    for c in range(ncores):
        maps.append({"xin": xT[c * NSEQ:(c + 1) * NSEQ], "wall": wall, "ctab": ctab, "cdt": cdt,
                     "cmask": cmask, "csm": csm, "ctri": ctri})
    return maps


def unpack_out(res, NSEQ):
    outs = []
    for r in res:
        o = np.asarray(r["out"]).reshape(NSEQ, 128, 8, SEQ)
        outs.append(o.transpose(0, 3, 2, 1).reshape(NSEQ, SEQ, D))
    return np.concatenate(outs, 0)


def kernel(x, w_in, p_ret, p_sb, w_out, w_up, w_down, g_mix, g_mlp, g_final):
    NSEQ = x.shape[0] // NCORES
    nc, _ = build(DEPTH, NSEQ)
    maps = make_inputs(x, w_in, p_ret, p_sb, w_out, w_up, w_down, g_mix, g_mlp, g_final, DEPTH, NSEQ, NCORES)
    res = run_bass_kernel_spmd(nc, maps, core_ids=list(range(NCORES)))
    return unpack_out(res.results, NSEQ).astype(np.float32)
```

```python
import contextlib
import numpy as np
import concourse.bass as bass
import concourse.mybir as mybir
from concourse.bass_utils import run_bass_kernel_spmd

F32 = mybir.dt.float32
BF16 = mybir.dt.bfloat16
ALU = mybir.AluOpType
AF = mybir.ActivationFunctionType
AX = mybir.AxisListType

PE, ACT, DVE, POOL, SP = "pe", "act", "dve", "pool", "sp"
SEM_LIMIT = 60000
DMA_LIMIT = 1800


class V:
    __slots__ = ("ap", "buf", "p0", "p1", "lo", "hi")

    def __init__(self, ap, buf, p0, p1, lo, hi):
        self.ap, self.buf, self.p0, self.p1, self.lo, self.hi = ap, buf, p0, p1, lo, hi

    def re(self, pattern, **kw):
        return V(self.ap.rearrange(pattern, **kw), self.buf, self.p0, self.p1, self.lo, self.hi)

    def with_ap(self, ap):
        return V(ap, self.buf, self.p0, self.p1, self.lo, self.hi)


class Buf:
    def __init__(self, name, handle, P, F, dtype, is_psum=False):
        self.name, self.h, self.P, self.F, self.dtype = name, handle, P, F, dtype
        self.is_psum = is_psum
        self.recs = []

    def v(self, lo=0, hi=None, p0=0, p1=None, f32=False):
        if hi is None:
            hi = self.F
        if p1 is None:
            p1 = self.P
        assert 0 <= lo < hi <= self.F and 0 <= p0 < p1 <= self.P, (self.name, lo, hi, p0, p1)
        ap = self.h[p0:p1, lo:hi]
        if f32:
            assert lo % 2 == 0 and hi % 2 == 0
            ap = ap.bitcast(F32)
        return V(ap, self, p0, p1, lo, hi)


class DmaSem:
    def __init__(self, sched, name):
        self.sched, self.name = sched, name
        self.hw = [sched._new_sem(name + "_0")]
        self.count = 0
        self.finals = []
        self.closed = 0
        self.consumers = []


class Op:
    __slots__ = ("eng", "fn", "deps", "dsem", "dsem_hw", "dsem_val", "signal", "sigval", "is_dma")

    def __init__(self, eng, fn, deps, dsem=None):
        self.eng, self.fn, self.deps, self.dsem = eng, fn, deps, dsem
        self.is_dma = dsem is not None
        self.signal = False
        self.sigval = None
        self.dsem_hw = None
        self.dsem_val = None


class Sched:
    def __init__(self, nc, stack):
        self.nc, self.stack = nc, stack
        self.ops = []
        self.bufs = []
        self.nsem = 0
        self.final_waits = []

    def _new_sem(self, name):
        self.nsem += 1
        return self.stack.enter_context(self.nc.semaphore(name))

    def sbuf(self, name, P, F, dtype):
        h = self.stack.enter_context(self.nc.sbuf_tensor(name, [P, F], dtype))
        b = Buf(name, h, P, F, dtype)
        self.bufs.append(b)
        return b

    def psum(self, name, P, F, dtype):
        h = self.stack.enter_context(self.nc.psum_tensor(name, [P, F], dtype))
        b = Buf(name, h, P, F, dtype, is_psum=True)
        self.bufs.append(b)
        return b

    def dma_sem(self, name):
        return DmaSem(self, name)

    def _access(self, v, n, eng, is_write, is_dma):
        deps = set()
        recs = v.buf.recs
        keep = []
        psum = v.buf.is_psum
        if psum:
            v = V(v.ap, v.buf, 0, v.buf.P, 0, v.buf.F)
        for r in recs:
            ov = not (r[1] <= v.p0 or v.p1 <= r[0] or r[3] <= v.lo or v.hi <= r[2])
            if ov and (r[5] or is_write or (psum and r[6] != eng)) and r[4] != n:
                deps.add(r[4])
            covered = ov and v.p0 <= r[0] and r[1] <= v.p1 and v.lo <= r[2] and r[3] <= v.hi
            if covered and r[4] != n:
                if is_write:
                    continue
                if (not r[5]) and (not is_dma) and r[6] == eng:
                    continue
            keep.append(r)
        keep.append([v.p0, v.p1, v.lo, v.hi, n, is_write, None if is_dma else eng])
        v.buf.recs = keep
        return deps

    def op(self, eng, fn, reads=(), writes=(), dsem=None):
        n = len(self.ops)
        is_dma = dsem is not None
        deps = set()
        for v in reads:
            deps |= self._access(v, n, eng, False, is_dma)
        for v in writes:
            deps |= self._access(v, n, eng, True, is_dma)
        deps.discard(n)
        o = Op(eng, fn, None, dsem)
        if is_dma:
            if dsem.consumers:
                deps |= set(dsem.consumers)
                dsem.consumers = []
                if dsem.count > DMA_LIMIT:
                    dsem.finals.append(dsem.count * 16)
                    dsem.hw.append(self._new_sem(dsem.name + "_%d" % len(dsem.hw)))
                    dsem.count = 0
                dsem.closed = dsem.count
            dsem.count += 1
            o.dsem_hw = len(dsem.hw) - 1
            o.dsem_val = dsem.count * 16
        final = []
        for d in deps:
            p = self.ops[d]
            if p.is_dma:
                if p.dsem_hw != len(p.dsem.hw) - 1:
                    final.append(("d", p.dsem, p.dsem_hw, p.dsem.finals[p.dsem_hw]))
                    continue
                if p.dsem is dsem:
                    if p.dsem_val > dsem.closed * 16:
                        final.append(("d", p.dsem, p.dsem_hw, (dsem.count - 1) * 16))
                    continue
                final.append(("d", p.dsem, p.dsem_hw, p.dsem.count * 16))
                p.dsem.consumers.append(n)
            else:
                if p.eng == PE and eng == PE:
                    continue
                p.signal = True
                final.append(("e", d))
        o.deps = final
        self.ops.append(o)
        return n

    def pe(self, fn, reads=(), writes=()):
        return self.op(PE, fn, reads, writes)

    def act(self, fn, reads=(), writes=()):
        return self.op(ACT, fn, reads, writes)

    def dve(self, fn, reads=(), writes=()):
        return self.op(DVE, fn, reads, writes)

    def pool(self, fn, reads=(), writes=()):
        return self.op(POOL, fn, reads, writes)

    def dma(self, eng, dsem, out, in_, reads=(), writes=()):
        return self.op(eng, lambda e: e.dma_start(out=out, in_=in_), reads, writes, dsem=dsem)

    def emit(self, out_sems=()):
        nc = self.nc
        engs = [PE, ACT, DVE, POOL, SP]
        ops = self.ops
        per_eng = {e: [] for e in engs}
        pos = {}
        for i, o in enumerate(ops):
            pos[i] = len(per_eng[o.eng])
            per_eng[o.eng].append(i)

        plan = {}
        for e in engs:
            wp = {}
            for i in per_eng[e]:
                best = {}
                for d in ops[i].deps:
                    if d[0] == "e":
                        p = ops[d[1]]
                        if p.eng not in best or pos[d[1]] > pos[best[p.eng]]:
                            best[p.eng] = d[1]
                lst = []
                for pe_, pi in best.items():
                    if wp.get(pe_, -1) >= pos[pi]:
                        continue
                    wp[pe_] = pos[pi]
                    lst.append(pi)
                plan[i] = lst
        for o in ops:
            o.signal = False
        for i, lst in plan.items():
            for pi in lst:
                ops[pi].signal = True
        counts = {e: 0 for e in engs}
        for o in ops:
            if o.signal and not o.is_dma:
                counts[o.eng] += 1
                o.sigval = counts[o.eng]
        esems = {}
        for e in engs:
            nchunks = (counts[e] + SEM_LIMIT - 1) // SEM_LIMIT
            esems[e] = [self._new_sem("sig_%s_%d" % (e, k)) for k in range(max(nchunks, 1))]
        stats = {e: [len(per_eng[e]), 0, counts[e]] for e in engs}

        def replay(eng_name, eobj):
            waited = {}
            for i in per_eng[eng_name]:
                o = ops[i]
                need = {}
                for d in o.deps:
                    if d[0] == "d":
                        key = ("d", id(d[1]), d[2])
                        sem = d[1].hw[d[2]]
                        val = d[3]
                        if waited.get(key, 0) >= val:
                            continue
                        if key not in need or need[key][1] < val:
                            need[key] = (sem, val)
                for pi in plan[i]:
                    p = ops[pi]
                    ch = (p.sigval - 1) // SEM_LIMIT
                    need[("e", p.eng, ch, pi)] = (esems[p.eng][ch], (p.sigval - 1) % SEM_LIMIT + 1)
                for key, (sem, val) in need.items():
                    eobj.wait_ge(sem, val)
                    if key[0] == "d":
                        waited[key] = val
                    stats[eng_name][1] += 1
                ins = o.fn(eobj)
                if o.is_dma:
                    ins.then_inc(o.dsem.hw[o.dsem_hw], 16)
                elif o.signal:
                    ch = (o.sigval - 1) // SEM_LIMIT
                    ins.then_inc(esems[o.eng][ch], 1)
            if eng_name == SP:
                for ds in out_sems:
                    eobj.wait_ge(ds.hw[-1], ds.count * 16)

        with nc.Block() as block:
            @block.tensor
            def _(e):
                replay(PE, e)

            @block.scalar
            def _(e):
                replay(ACT, e)

            @block.vector
            def _(e):
                replay(DVE, e)

            @block.gpsimd
            def _(e):
                replay(POOL, e)

            @block.sync
            def _(e):
                replay(SP, e)
        return stats


D = 1024
SEQ = 2048
DEPTH = 4
NCORES = 8
NG = 42
GW = 4096
EPS = 1e-6
GN_EPS = 1e-5
T = 512
PARTIAL = True
GAMMAS = [1.0 - 2.0 ** (-5.0 - h) for h in range(4)]

X_OFF = 0
HT_OFF = 32768
R1_OFF = 49152
R2_OFF = 65536
W_OFF = [81920, 86016]
SCR = 90112
CST = 104960
ARENA = 105984
C_GMIX, C_GMLP, C_GFIN, C_KDEC, C_QDEC, NSM = 0, 32, 64, 72, 76, 80
TRI_OFF, ONES_OFF, ID_OFF, NTRI_OFF = CST + 160, CST + 288, CST + 416, CST + 544


def build(NL, NSEQ, stop=None):
    nc = bass.Bass("TRN2", target_bir_lowering=False)
    xin = nc.dram_tensor("xin", [NSEQ, 128, 8 * SEQ], F32, kind="ExternalInput").ap()
    wall = nc.dram_tensor("wall", [NL, NG, 128, GW], F32, kind="ExternalInput").ap()
    ctab = nc.dram_tensor("ctab", [128, 2 * SEQ], F32, kind="ExternalInput").ap()
    cdt = nc.dram_tensor("cdt", [128, 4 * 512], F32, kind="ExternalInput").ap()
    cmask = nc.dram_tensor("cmask", [128, 4 * 512], F32, kind="ExternalInput").ap()
    csm = nc.dram_tensor("csm", [128, NSM], F32, kind="ExternalInput").ap()
    ctri = nc.dram_tensor("ctri", [128, 4 * 128], F32, kind="ExternalInput").ap()
    out = nc.dram_tensor("out", [NSEQ, 128, 8 * SEQ], F32, kind="ExternalOutput").ap()
    dbg = nc.dram_tensor("dbg", [128, ARENA], BF16, kind="ExternalOutput").ap() if stop is not None else None

    with contextlib.ExitStack() as st:
        S = Sched(nc, st)
        A = S.sbuf("arena", 128, ARENA, BF16)
        PB = [S.psum("pb%d" % i, 128, 512, F32) for i in range(7)]
        PT = S.psum("pt", 128, 1024, BF16)
        rot = [0]

        def rb():
            rot[0] = (rot[0] + 1) % 5
            return PB[rot[0]]

        wsem = [S.dma_sem("w0"), S.dma_sem("w1")]
        xsem = S.dma_sem("x")
        csem = S.dma_sem("c")
        cpsem = S.dma_sem("cp")
        tsem = [S.dma_sem("t0"), S.dma_sem("t1")]
        osem = S.dma_sem("o")

        def bv(off, n):
            return A.v(off, off + n)

        def fv(off, n):
            return A.v(off, off + 2 * n, f32=True)

        def Xv(k, c0, n):
            return fv(X_OFF + 2 * (k * SEQ + c0), n)

        def hT(k, c0, n):
            return bv(HT_OFF + k * SEQ + c0, n)

        def cs(col):
            return fv(CST + 2 * col, 1)

        TRI = bv(TRI_OFF, 128)
        ONES = bv(ONES_OFF, 128)
        IDENT = bv(ID_OFF, 128)
        NTRI = bv(NTRI_OFF, 128)

        def mm(ps, lhsT, rhs, start, stop, skip=False):
            S.pe(lambda e: e.matmul(ps.ap, lhsT=lhsT.ap, rhs=rhs.ap, start=start, stop=stop, skip_group_check=skip),
                 reads=[lhsT, rhs], writes=[ps])

        def tr(ps, in_):
            S.pe(lambda e: e.transpose(ps.ap, in_.ap, IDENT.ap), reads=[in_, IDENT], writes=[ps])

        def act(o, i, func, scale=1.0, bias=0.0):
            reads = [i]
            sc, bi = scale, bias
            if isinstance(scale, V):
                reads.append(scale)
                sc = scale.ap
            if isinstance(bias, V):
                reads.append(bias)
                bi = bias.ap
            S.act(lambda e: e.activation(out=o.ap, in_=i.ap, func=func, scale=sc, bias=bi), reads=reads, writes=[o])

        def tt(o, a, b, op):
            S.dve(lambda e: e.tensor_tensor(out=o.ap, in0=a.ap, in1=b.ap, op=op), reads=[a, b], writes=[o])

        def ts(o, a, s1, s2, op0, op1=None):
            reads = [a]
            v1, v2 = s1, s2
            if isinstance(s1, V):
                reads.append(s1)
                v1 = s1.ap
            if isinstance(s2, V):
                reads.append(s2)
                v2 = s2.ap
            if op1 is None:
                S.dve(lambda e: e.tensor_scalar(out=o.ap, in0=a.ap, scalar1=v1, scalar2=None, op0=op0),
                      reads=reads, writes=[o])
            else:
                S.dve(lambda e: e.tensor_scalar(out=o.ap, in0=a.ap, scalar1=v1, scalar2=v2, op0=op0, op1=op1),
                      reads=reads, writes=[o])

        def stt(o, a, sc, b, op0, op1):
            reads = [a, b]
            v = sc
            if isinstance(sc, V):
                reads.append(sc)
                v = sc.ap
            S.dve(lambda e: e.scalar_tensor_tensor(out=o.ap, in0=a.ap, scalar=v, in1=b.ap, op0=op0, op1=op1),
                  reads=reads, writes=[o])

        def cpy(o, i):
            S.dve(lambda e: e.tensor_copy(out=o.ap, in_=i.ap), reads=[i], writes=[o])

        def sigmoid_to(o, ps, t1, t2):
            act(t1, ps, AF.Exp, scale=-1.0)
            act(t2, t1, AF.Ln, bias=1.0)
            act(o, t2, AF.Exp, scale=-1.0)

        wstate = {"next": 0, "list": []}
        for l in range(NL):
            for g in range(NG):
                wstate["list"].append((l, g))
        wstate["list"] = wstate["list"] * NSEQ
        nW = len(wstate["list"])
        wcnt = [0]

        def issue_w(i):
            l, g = wstate["list"][i]
            sl = i % 2
            dst = bv(W_OFF[sl], GW)
            S.dma(POOL, wsem[sl], dst.ap, wall[l, g], writes=[dst])

        def next_w():
            i = wcnt[0]
            if i == 0:
                issue_w(0)
            if i + 1 < nW:
                issue_w(i + 1)
            wcnt[0] += 1
            return W_OFF[i % 2]

        d0 = fv(CST, NSM)
        S.dma(SP, csem, d0.ap, csm, writes=[d0])
        d1 = bv(TRI_OFF, 512)
        S.dma(POOL, cpsem, d1.ap, ctri, writes=[d1])

        def rmsnorm(gbase, dst_fn, after=None):
            sqb = lambda k: bv(SCR + k * 512, 512)
            lnt = fv(SCR + 4096, 512)
            rstd = fv(SCR + 5120, 512)
            for t in range(4):
                c0 = t * 512
                for k in range(8):
                    act(sqb(k), Xv(k, c0, 512), AF.Square)
                ps = rb().v()
                for k in range(8):
                    mm(ps, ONES, sqb(k), k == 0, k == 7)
                act(lnt, ps, AF.Ln, scale=1.0 / D, bias=EPS)
                act(rstd, lnt, AF.Exp, scale=-0.5)
                for k in range(8):
                    stt(dst_fn(k, t), Xv(k, c0, 512), cs(gbase + k), rstd, ALU.mult, ALU.mult)
                if after is not None:
                    after(t)

        def retention_head(hh):
            qT = lambda c0, n: bv(R2_OFF + c0, n)
            kT = lambda c0, n: bv(R2_OFF + 2048 + c0, n)
            ktok = lambda n0, nn: bv(R2_OFF + 4096 + n0 * 128, nn * 128)
            vtok = lambda n0, nn: bv(R2_OFF + 6144 + n0 * 256, nn * 256)
            sg = lambda n0, nn: bv(R2_OFF + 10240 + n0 * 256, nn * 256)
            y4 = lambda j0, nj: fv(R2_OFF + 14336 + 2 * j0 * 256, nj * 256)
            cosb = lambda i: fv(SCR + i * 1024, 512)
            sinb = lambda i: fv(SCR + 2048 + i * 1024, 512)
            DT = fv(SCR + 4096, 512)
            tmp1 = fv(SCR + 5120, 512)
            tmp2 = fv(SCR + 6144, 512)
            Sf = lambda i: fv(SCR + 7168 + i * 512, 256)
            Sb = lambda n: bv(SCR + 8192 + n * 256, 256)
            scT = lambda i: bv(SCR + 12288 + i * 512, 512)
            rtok = bv(SCR + 13312, 1024)
            bnst = lambda j: fv(SCR + 14336 + j * 12, 6)
            mv = lambda j: fv(SCR + 14336 + 48 + j * 4, 2)
            mvall = fv(SCR + 14336 + 48, 8)
            lnv = fv(SCR + 14336 + 64, 4)
            rs4 = fv(SCR + 14336 + 72, 4)
            kdec = cs(C_KDEC + hh)
            qdec = cs(C_QDEC + hh)
            gC = GAMMAS[hh] ** 128

            S.dma(SP, csem, DT.ap, cdt[:, hh * 512:(hh + 1) * 512], writes=[DT])
            w = next_w()
            wv = lambda k, c0, n: bv(w + k * 512 + c0, n)
            for t in range(4):
                c0 = t * 512
                cb, sb_ = cosb(t % 2), sinb(t % 2)
                S.dma(SP, tsem[t % 2], cb.ap, ctab[:, c0:c0 + 512], writes=[cb])
                S.dma(SP, tsem[t % 2], sb_.ap, ctab[:, SEQ + c0:SEQ + c0 + 512], writes=[sb_])
                for dst, col in ((qT, 0), (kT, 256)):
                    ps1 = rb().v()
                    for k in range(8):
                        mm(ps1, wv(k, col, 128), hT(k, c0, 512), k == 0, k == 7)
                    ps2 = rb().v()
                    for k in range(8):
                        mm(ps2, wv(k, col + 128, 128), hT(k, c0, 512), k == 0, k == 7)
                    tt(tmp1, ps1, cb, ALU.mult)
                    tt(tmp2, ps2, sb_, ALU.mult)
                    tt(dst(c0, 512), tmp1, tmp2, ALU.add)
            w = next_w()
            for t in range(4):
                for pr in range(2):
                    n0 = t * 4 + pr * 2
                    psv = rb()
                    for j in range(2):
                        o = psv.v(j * 256, (j + 1) * 256)
                        for k in range(8):
                            mm(o, hT(k, (n0 + j) * 128, 128), wv(k, 0, 256), k == 0, k == 7)
                    act(vtok(n0, 2), psv.v(), AF.Copy)
                    psg = rb()
                    for j in range(2):
                        o = psg.v(j * 256, (j + 1) * 256)
                        for k in range(8):
                            mm(o, hT(k, (n0 + j) * 128, 128), wv(k, 256, 256), k == 0, k == 7)
                    sigmoid_to(tmp1, psg.v(), tmp1, tmp2)
                    tt(sg(n0, 2), psg.v(), tmp1, ALU.mult)
            for t in range(4):
                for j in range(4):
                    tr(PT.v(j * 128, (j + 1) * 128), kT((t * 4 + j) * 128, 128))
                act(ktok(t * 4, 4), PT.v(0, 512), AF.Copy, scale=kdec)
            S.dve(lambda e: e.memset(Sb(0).ap, 0.0), writes=[Sb(0)])
            for n in range(15):
                ps = rb().v(0, 256)
                mm(ps, ktok(n, 1), vtok(n, 1), True, True)
                if n == 0:
                    cpy(Sf(0), ps)
                else:
                    stt(Sf(n % 2), Sf((n - 1) % 2), gC, ps, ALU.mult, ALU.add)
                act(Sb(n + 1), Sf(n % 2), AF.Copy)
            for t in range(4):
                pss = rb()
                for j in range(4):
                    n = t * 4 + j
                    mm(pss.v(j * 128, (j + 1) * 128), kT(n * 128, 128), qT(n * 128, 128), True, True)
                sc = scT(t % 2)
                tt(sc, pss.v(), DT, ALU.mult)
                for pr in range(2):
                    pso, psx = rb(), rb()
                    for j in range(2):
                        jj = pr * 2 + j
                        n = t * 4 + jj
                        mm(pso.v(j * 256, (j + 1) * 256), bv(SCR + 12288 + (t % 2) * 512 + jj * 128, 128), vtok(n, 1), True, True)
                        mm(psx.v(j * 256, (j + 1) * 256), qT(n * 128, 128), Sb(n), True, True)
                    act(tmp1, psx.v(), AF.Copy, scale=qdec)
                    tt(y4(pr * 2, 2), pso.v(), tmp1, ALU.add)
                for j in range(4):
                    S.dve(lambda e, j=j: e.bn_stats(out=bnst(j).ap, in_=y4(j, 1).ap), reads=[y4(j, 1)], writes=[bnst(j)])
                    S.dve(lambda e, j=j: e.bn_aggr(out=mv(j).ap, in_=bnst(j).ap), reads=[bnst(j)], writes=[mv(j)])
                mvar = mvall.with_ap(mvall.ap.rearrange("p (j c) -> p j c", c=2)[:, :, 1])
                act(lnv, mvar, AF.Ln, bias=GN_EPS)
                act(rs4, lnv, AF.Exp, scale=-0.5)
                for j in range(4):
                    n = t * 4 + j
                    tj = fv(SCR + 5120 + (j % 2) * 1024, 256)
                    ts(tj, y4(j, 1), fv(SCR + 14336 + 48 + j * 4, 1), fv(SCR + 14336 + 72 + j * 2, 1), ALU.subtract, ALU.mult)
                    tt(bv(SCR + 13312 + j * 256, 256), tj, sg(n, 1), ALU.mult)
                for e2 in range(2):
                    for j in range(4):
                        tr(PT.v((e2 * 4 + j) * 128, (e2 * 4 + j + 1) * 128), bv(SCR + 13312 + j * 256 + e2 * 128, 128))
                for e2 in range(2):
                    act(bv(R1_OFF + (2 * hh + e2) * SEQ + t * 512, 512), PT.v(e2 * 512, (e2 + 1) * 512), AF.Copy)

        def gated_proj(src_off, first):
            e1 = fv(SCR, 512)
            e2 = fv(SCR + 1024, 512)
            sgm = lambda i: fv(SCR + 2048 + i * 1024, 512)
            tb = fv(SCR + 4096, 512)
            for g in range(4):
                w = next_w()
                wv = lambda k, c0, n: bv(w + k * 512 + c0, n)
                for cc in range(2):
                    c = g * 2 + cc
                    for t in range(4):
                        psa, psg = rb().v(), rb().v()
                        for k in range(8):
                            mm(psa, wv(k, cc * 128, 128), bv(src_off + k * SEQ + t * 512, 512), k == 0, k == 7)
                        for k in range(8):
                            mm(psg, wv(k, 256 + cc * 128, 128), hT(k, t * 512, 512), k == 0, k == 7)
                        sm = sgm(t % 2)
                        sigmoid_to(sm, psg, e1, e2)
                        ga = bv(R2_OFF + c * SEQ + t * 512, 512)
                        if first:
                            tt(ga, psa, sm, ALU.mult)
                        else:
                            tt(tb, psa, sm, ALU.mult)
                            tt(ga, ga, tb, ALU.add)

        def sb_head(hh):
            qT = lambda c0, n: bv(SCR + c0, n)
            kT = lambda c0, n: bv(SCR + 2048 + c0, n)
            vtok = lambda n0, nn: bv(SCR + 4096 + n0 * 128, nn * 128)
            E = lambda lo: fv(SCR + 6144 + 2 * lo, 512 - lo)
            Lp = lambda i, lo: bv(SCR + 7168 + i * 512 + lo, 512 - lo)
            tmp = lambda lo: fv(SCR + 8704 + 2 * lo, 512 - lo)
            Wt = lambda i, lo: bv(SCR + 9728 + i * 512 + lo, 512 - lo)
            R = lambda i, lo: fv(SCR + 10752 + i * 1024 + 2 * lo, 512 - lo)
            M = lambda j, lo: bv(SCR + 12800 + j * 512 + lo, 512 - lo)
            scale = 128.0 ** -0.5
            w = next_w()
            wv = lambda k, c0, n: bv(w + k * 384 + c0, n)
            for t in range(4):
                psq = rb().v()
                for k in range(8):
                    mm(psq, wv(k, 0, 128), hT(k, t * 512, 512), k == 0, k == 7)
                act(qT(t * 512, 512), psq, AF.Copy, scale=scale)
                psk = rb().v()
                for k in range(8):
                    mm(psk, wv(k, 128, 128), hT(k, t * 512, 512), k == 0, k == 7)
                cpy(kT(t * 512, 512), psk)
                psv = rb()
                for j in range(4):
                    o = psv.v(j * 128, (j + 1) * 128)
                    for k in range(8):
                        mm(o, hT(k, (t * 4 + j) * 128, 128), wv(k, 256, 128), k == 0, k == 7)
                cpy(vtok(t * 4, 4), psv.v())
            blocks = []
            for i in range(4):
                nblk = 4 * i + 4
                for bi, sbk in enumerate(range(nblk - 1, -1, -1)):
                    j = sbk - 4 * i
                    lo = (128 * j if j > 0 else 0) if PARTIAL else 0
                    blocks.append(dict(i=i, bi=bi, sbk=sbk, j=j, lo=lo, diag=(j >= 0), last=(bi == nblk - 1), n=len(blocks)))

            def stage1(b):
                lo, i = b["lo"], b["i"]
                psz = rb()
                b["lp"] = Lp(b["n"] % 3, lo)
                mm(psz.v(lo, 512), kT(b["sbk"] * 128, 128), qT(i * 512 + lo, 512 - lo), True, True)
                act(E(lo), psz.v(lo, 512), AF.Exp)
                act(b["lp"], E(lo), AF.Ln, bias=1.0)
                if b["diag"]:
                    tt(b["lp"], b["lp"], M(b["j"], lo), ALU.mult)

            def stage2(b):
                lo, i = b["lo"], b["i"]
                if b["bi"] == 0:
                    rr = R(i % 2, 0)
                    S.dve(lambda e: e.memset(rr.ap, 0.0), writes=[rr])
                psc = rb()
                mm(psc.v(lo, 512), NTRI, b["lp"], True, False)
                mm(psc.v(lo, 512), kT(b["sbk"] * 128, 128), qT(i * 512 + lo, 512 - lo), False, True)
                if not b["last"]:
                    psr = rb()
                    mm(psr.v(lo, 512), ONES, b["lp"], True, True)
                b["wt"] = Wt(b["n"] % 2, lo)
                if b["bi"] == 0:
                    act(b["wt"], psc.v(lo, 512), AF.Exp)
                else:
                    tt(tmp(lo), psc.v(lo, 512), R(i % 2, lo), ALU.subtract)
                    act(b["wt"], tmp(lo), AF.Exp)
                if not b["last"]:
                    tt(R(i % 2, lo), psr.v(lo, 512), R(i % 2, lo), ALU.add)
                if b["diag"]:
                    tt(b["wt"], b["wt"], M(b["j"], lo), ALU.mult)

            def stage3(b):
                lo, i = b["lo"], b["i"]
                pso = PB[5 + (i % 2)]
                mm(pso.v(lo, 512), vtok(b["sbk"], 1), b["wt"], b["bi"] == 0, b["last"], skip=True)
                if b["last"]:
                    act(bv(R1_OFF + hh * SEQ + i * 512, 512), pso.v(), AF.Copy)

            nb = len(blocks)
            for s_ in range(nb + 2):
                if s_ < nb:
                    stage1(blocks[s_])
                if 0 <= s_ - 1 < nb:
                    stage2(blocks[s_ - 1])
                if 0 <= s_ - 2 < nb:
                    stage3(blocks[s_ - 2])

        def out_proj():
            for g in range(2):
                w = next_w()
                wv = lambda k, c0, n: bv(w + k * 512 + c0, n)
                for cc in range(4):
                    c = g * 4 + cc
                    for t in range(4):
                        ps = rb().v()
                        for k in range(8):
                            mm(ps, wv(k, cc * 128, 128), bv(R2_OFF + k * SEQ + t * 512, 512), k == 0, k == 7)
                        tt(Xv(c, t * 512, 512), ps, Xv(c, t * 512, 512), ALU.add)

        def mlp():
            upT = lambda kk, c0, n: bv(R1_OFF + kk * SEQ + c0, n)
            sq = lambda i: fv(SCR + i * 1024, 512)
            it = 0
            for hf in range(2):
                for g in range(4):
                    w = next_w()
                    wv = lambda k, c0, n: bv(w + k * 512 + c0, n)
                    for jj in range(4):
                        for t in range(4):
                            ps = rb().v()
                            for k in range(8):
                                mm(ps, wv(k, jj * 128, 128), hT(k, t * 512, 512), k == 0, k == 7)
                            s_ = sq(it % 2)
                            it += 1
                            act(s_, ps, AF.Square)
                            stt(upT(g * 4 + jj, t * 512, 512), ps, 0.0, s_, ALU.is_gt, ALU.mult)
                for cg in range(4):
                    w = next_w()
                    wv = lambda k, c0, n: bv(w + k * 256 + c0, n)
                    for cc in range(2):
                        c = cg * 2 + cc
                        for t in range(4):
                            ps = rb().v()
                            for k in range(16):
                                mm(ps, wv(k, cc * 128, 128), upT(k, t * 512, 512), k == 0, k == 15)
                            tt(Xv(c, t * 512, 512), ps, Xv(c, t * 512, 512), ALU.add)

        class _Stop(Exception):
            pass

        def chk(i):
            if stop == i:
                whole = A.v()
                S.dma(SP, osem, dbg, whole.ap, reads=[whole])
                raise _Stop()

        try:
            for s in range(NSEQ):
                for k in range(8):
                    d = Xv(k, 0, SEQ)
                    S.dma(SP, xsem, d.ap, xin[s, :, k * SEQ:(k + 1) * SEQ], writes=[d])
                for l in range(NL):
                    rmsnorm(C_GMIX + l * 8, lambda k, t: hT(k, t * 512, 512))
                    chk(1)
                    for hh in range(4):
                        retention_head(hh)
                    chk(2)
                    gated_proj(R1_OFF, True)
                    chk(3)
                    mk = bv(SCR + 12800, 2048)
                    S.dma(POOL, cpsem, mk.ap, cmask, writes=[mk])
                    for hh in range(8):
                        sb_head(hh)
                    chk(4)
                    gated_proj(R1_OFF, False)
                    chk(5)
                    out_proj()
                    chk(6)
                    rmsnorm(C_GMLP + l * 8, lambda k, t: hT(k, t * 512, 512))
                    mlp()
                    chk(7)
                ost = lambda t, k: fv(HT_OFF + (t % 2) * 8192 + 2 * k * 512, 512)

                def store(t, s=s):
                    src = fv(HT_OFF + (t % 2) * 8192, 4096)
                    dst = out[s].rearrange("p (k t) -> p k t", k=8)[:, :, t * 512:(t + 1) * 512]
                    S.dma(SP, osem, dst, src.ap.rearrange("p (k t) -> p k t", k=8), reads=[src])

                rmsnorm(C_GFIN, lambda k, t: ost(t, k), after=store)
        except _Stop:
            pass
        stats = S.emit(out_sems=[osem])
    return nc, stats


def _pack(M):
    K, n = M.shape
    return np.ascontiguousarray(M.reshape(K // 128, 128, n).transpose(1, 0, 2).reshape(128, (K // 128) * n))


def _pad(a):
    o = np.zeros((128, GW), np.float32)
    o[:, :a.shape[1]] = a
    return o


def pack_weights(w_in, p_ret, p_sb, w_out, w_up, w_down, NL):
    wall = np.zeros((NL, NG, 128, GW), np.float32)
    OQ, OK_, OV, OG, SQ, SK, SV, GA, GB = 0, 512, 1024, 2048, 3072, 4096, 5120, 6144, 7168
    for l in range(NL):
        wi = w_in[l]
        g = 0
        for hh in range(4):
            q = wi[:, OQ + hh * 128:OQ + (hh + 1) * 128]
            k = wi[:, OK_ + hh * 128:OK_ + (hh + 1) * 128]
            sw = lambda m: np.concatenate([m[:, 64:], m[:, :64]], 1)
            wall[l, g] = _pack(np.concatenate([q, sw(q), k, sw(k)], 1)); g += 1
            v = wi[:, OV + hh * 256:OV + (hh + 1) * 256]
            gg = wi[:, OG + hh * 256:OG + (hh + 1) * 256]
            wall[l, g] = _pack(np.concatenate([v, gg], 1)); g += 1
        for gi in range(4):
            wall[l, g] = _pack(np.concatenate([p_ret[l][:, gi * 256:(gi + 1) * 256],
                                               wi[:, GA + gi * 256:GA + (gi + 1) * 256]], 1)); g += 1
        for hh in range(8):
            m = np.concatenate([wi[:, SQ + hh * 128:SQ + (hh + 1) * 128], wi[:, SK + hh * 128:SK + (hh + 1) * 128],
                                wi[:, SV + hh * 128:SV + (hh + 1) * 128]], 1)
            wall[l, g] = _pad(_pack(m)); g += 1
        for gi in range(4):
            wall[l, g] = _pack(np.concatenate([p_sb[l][:, gi * 256:(gi + 1) * 256],
                                               wi[:, GB + gi * 256:GB + (gi + 1) * 256]], 1)); g += 1
        for gi in range(2):
            wall[l, g] = _pack(w_out[l][:, gi * 512:(gi + 1) * 512]); g += 1
        for hf in range(2):
            for gi in range(4):
                wall[l, g] = _pack(w_up[l][:, hf * 2048 + gi * 512:hf * 2048 + (gi + 1) * 512]); g += 1
            for cg in range(4):
                wall[l, g] = _pack(w_down[l][hf * 2048:(hf + 1) * 2048, cg * 256:(cg + 1) * 256]); g += 1
        assert g == NG
    return wall


def const_tables():
    p = np.arange(128)
    half = 64
    inv_freq = (10000.0 ** (-(np.arange(half, dtype=np.float32)) / half)).astype(np.float32)
    pos = np.arange(SEQ, dtype=np.float32)
    ang = pos[None, :] * inv_freq[p % 64][:, None]
    cos = np.cos(ang).astype(np.float32)
    sin = np.sin(ang).astype(np.float32)
    sinS = np.where((p < 64)[:, None], -sin, sin).astype(np.float32)
    ctab = np.concatenate([cos, sinS], 1).astype(np.float32)
    idx = np.arange(128, dtype=np.float64)
    cdt = np.zeros((128, 4 * 512), np.float32)
    csm_dec = np.zeros((128, 8), np.float32)
    for h in range(4):
        lg = np.log1p(-(2.0 ** (-5.0 - h)))
        diff = idx[None, :] - idx[:, None]
        dt = np.where(diff >= 0, np.exp(lg * np.maximum(diff, 0.0)), 0.0) * (128.0 ** -0.5)
        cdt[:, h * 512:(h + 1) * 512] = np.tile(dt, (1, 4))
        csm_dec[:, h] = np.exp(lg * (127.0 - idx)) * (128.0 ** -0.5)
        csm_dec[:, 4 + h] = np.exp(lg * (idx + 1.0))
    cmask = np.zeros((128, 4 * 512), np.float32)
    tl = np.arange(512)
    for j in range(4):
        cmask[:, j * 512:(j + 1) * 512] = ((128 * j + p)[:, None] < tl[None, :]).astype(np.float32)
    tri = (p[:, None] >= p[None, :]).astype(np.float32)
    ctri = np.concatenate([tri, np.ones((128, 128), np.float32), np.eye(128, dtype=np.float32), -tri], 1)
    return ctab, cdt, cmask, csm_dec, ctri


def make_inputs(x, w_in, p_ret, p_sb, w_out, w_up, w_down, g_mix, g_mlp, g_final, NL, NSEQ, ncores):
    x = np.asarray(x, np.float32)
    B = x.shape[0]
    xT = np.ascontiguousarray(x.reshape(B, SEQ, 8, 128).transpose(0, 3, 2, 1)).reshape(B, 128, 8 * SEQ)
    wall = pack_weights(np.asarray(w_in), np.asarray(p_ret), np.asarray(p_sb), np.asarray(w_out),
                        np.asarray(w_up), np.asarray(w_down), NL)
    ctab, cdt, cmask, dec, ctri = const_tables()
    csm = np.zeros((128, NSM), np.float32)
    gm = np.asarray(g_mix, np.float32).reshape(DEPTH, 8, 128)
    gl = np.asarray(g_mlp, np.float32).reshape(DEPTH, 8, 128)
    gf = np.asarray(g_final, np.float32).reshape(8, 128)
    for l in range(DEPTH):
        csm[:, C_GMIX + l * 8:C_GMIX + (l + 1) * 8] = gm[l].T
        csm[:, C_GMLP + l * 8:C_GMLP + (l + 1) * 8] = gl[l].T
    csm[:, C_GFIN:C_GFIN + 8] = gf.T
    csm[:, C_KDEC:C_KDEC + 8] = dec
    maps = []
    for c in range(ncores):
        maps.append({"xin": xT[c * NSEQ:(c + 1) * NSEQ], "wall": wall, "ctab": ctab, "cdt": cdt,
                     "cmask": cmask, "csm": csm, "ctri": ctri})
    return maps


def unpack_out(res, NSEQ):
    outs = []
    for r in res:
        o = np.asarray(r["out"]).reshape(NSEQ, 128, 8, SEQ)
        outs.append(o.transpose(0, 3, 2, 1).reshape(NSEQ, SEQ, D))
    return np.concatenate(outs, 0)


def kernel(x, w_in, p_ret, p_sb, w_out, w_up, w_down, g_mix, g_mlp, g_final):
    NSEQ = x.shape[0] // NCORES
    nc, _ = build(DEPTH, NSEQ)
    maps = make_inputs(x, w_in, p_ret, p_sb, w_out, w_up, w_down, g_mix, g_mlp, g_final, DEPTH, NSEQ, NCORES)
    res = run_bass_kernel_spmd(nc, maps, core_ids=list(range(NCORES)))
    return unpack_out(res.results, NSEQ).astype(np.float32)
```
